# Optimizing a Trainium2 kernel written in Bass

```python
import jax
import jax.numpy as jnp
from jax import lax
import numpy as np

D_MODEL = 2048
BATCH = 4
SEQ = 4096
DEPTH = 4

GRID_W = 64
Q_BLOCK = 128
NEG_INF = -1e30
LN_EPS = 1e-5

A_HEADS = 8
A_KV_HEADS = 2
A_HEAD_DIM = 128
ROPE_THETA = 10000.0
QK_EPS = 1e-6

B_HEAD_DIM = 64
B_WIDTH = 1024
B_HEADS = B_WIDTH // B_HEAD_DIM
DECAY_LORA = 64
ICLR_LORA = 64
GATE_LORA = 128
GN_EPS = 64e-5

C_PATTERNS = ((128, 1), (512, 4), (2048, 16))
C_GROUPS = 3
C_HEADS_PER_GROUP = 4
C_HEADS = C_GROUPS * C_HEADS_PER_GROUP
C_HEAD_DIM = 128
REL_BUCKETS = 32
REL_MAX_DISTANCE = 1024

N_GROUPS = 8
EXPERTS_PER_GROUP = 8
N_EXPERTS = N_GROUPS * EXPERTS_PER_GROUP
TOP_K = 2
EXPERT_FF = 384
MOE_BLOCK = 128

A_Q = A_HEADS * A_HEAD_DIM
A_KV = A_KV_HEADS * A_HEAD_DIM
A_COLS = A_Q + 2 * A_KV
B_COLS = 3 * B_WIDTH + 2 * DECAY_LORA + 2 * ICLR_LORA + GATE_LORA
C_QKV = C_HEADS * C_HEAD_DIM
C_COLS = 3 * C_QKV
N_BRANCHES = 3
GATE_COLS = N_BRANCHES * D_MODEL
IN_COLS = A_COLS + B_COLS + C_COLS + GATE_COLS
A_OUT = A_Q
B_OUT = B_WIDTH
C_OUT = C_HEADS_PER_GROUP * C_HEAD_DIM
BRANCH_ROWS = A_OUT + B_OUT + C_OUT

kernel_name = 'hybrid_gated_bidir_encoder'


def _split(t, sizes):
    return jnp.split(t, np.cumsum(sizes)[:-1].tolist(), axis=-1)


def layer_norm(x, g, b):
    xf = x.astype(jnp.float32)
    mu = jnp.mean(xf, -1, keepdims=True)
    var = jnp.mean(jnp.square(xf - mu), -1, keepdims=True)
    return ((xf - mu) * lax.rsqrt(var + LN_EPS) * g + b).astype(x.dtype)


def rms_heads(t, gain):
    t = t.astype(jnp.float32)
    return t * lax.rsqrt(jnp.mean(t * t, -1, keepdims=True) + QK_EPS) * gain.astype(jnp.float32)


def rope_1d(t, pos):
    n = t.shape[-1]
    inv = ROPE_THETA ** (-jnp.arange(0, n, 2, dtype=jnp.float32) / n)
    ang = pos.astype(jnp.float32)[:, None] * inv[None, :]
    cos, sin = jnp.cos(ang)[:, None, :], jnp.sin(ang)[:, None, :]
    t1, t2 = t[..., : n // 2], t[..., n // 2:]
    return jnp.concatenate([t1 * cos - t2 * sin, t1 * sin + t2 * cos], -1)


def gqa_axial_attention(q, k, v, q_gain, k_gain):
    bsz, seq = q.shape[:2]
    rows = seq // GRID_W
    row = jnp.repeat(jnp.arange(rows), GRID_W)
    col = jnp.arange(seq) % GRID_W
    half = A_HEAD_DIM // 2

    def axial(t):
        return jnp.concatenate([rope_1d(t[..., :half], row), rope_1d(t[..., half:], col)], -1)

    q = axial(rms_heads(q, q_gain)) * A_HEAD_DIM ** -0.5
    k = axial(rms_heads(k, k_gain))
    v = v.astype(jnp.float32)
    grp = A_HEADS // A_KV_HEADS
    nblk = seq // Q_BLOCK
    qb = jnp.moveaxis(q.reshape(bsz, nblk, Q_BLOCK, A_KV_HEADS, grp, A_HEAD_DIM), 1, 0)

    def block(qblk):
        p = jax.nn.softmax(jnp.einsum('bqkgd,bskd->bkgqs', qblk, k), axis=-1)
        return jnp.einsum('bkgqs,bskd->bqkgd', p, v)

    o = lax.map(block, qb)
    return jnp.moveaxis(o, 0, 1).reshape(bsz, seq, A_Q)


def rwkv7_bidirectional(y, mu_prev, mu_next, w0, w2, a0, a2, g2, k_k, k_a, r_k, gn_g, gn_b):
    f32 = jnp.float32
    y = y.astype(f32)
    bsz, seq = y.shape[:2]
    prev = jnp.pad(y, ((0, 0), (1, 0), (0, 0)))[:, :-1]
    nxt = jnp.pad(y, ((0, 0), (0, 1), (0, 0)))[:, 1:]
    y = y + mu_prev * (prev - y) + mu_next * (nxt - y)
    r, k, v, hw, ha, hg = _split(y, [B_WIDTH] * 3 + [2 * DECAY_LORA, 2 * ICLR_LORA, GATE_LORA])
    hw = hw.reshape(bsz, seq, 2, DECAY_LORA)
    ha = ha.reshape(bsz, seq, 2, ICLR_LORA)
    log_w = -jax.nn.softplus(-(w0 + jnp.einsum('bsdr,drc->bsdc', jnp.tanh(hw), w2))) - 0.5
    decay = jnp.exp(-jnp.exp(log_w))
    a = jax.nn.sigmoid(a0 + jnp.einsum('bsdr,drc->bsdc', ha, a2))
    g = jax.nn.sigmoid(hg) @ g2

    def heads(t):
        return t.reshape(t.shape[:-1] + (B_HEADS, B_HEAD_DIM))

    kk = heads(k * k_k)
    kk = kk * lax.rsqrt(jnp.sum(kk * kk, -1, keepdims=True) + 1e-12)
    k_dir = heads(k[:, :, None] * (1.0 + (a - 1.0) * k_a))
    kk_a = kk[:, :, None] * heads(a)
    r_h, v_h = heads(r), heads(v)

    def per_dir(t):
        t = jnp.stack([t[:, :, 0], jnp.flip(t[:, :, 1], axis=1)], axis=0)
        return jnp.moveaxis(t, 2, 0)

    def shared(t):
        return per_dir(jnp.stack([t, t], axis=2))

    def step(state, inp):
        r_t, w_t, k_t, v_t, kk_t, b_t = inp
        sa = jnp.einsum('dbhvk,dbhk->dbhv', state, kk_t)
        state = (state * w_t[..., None, :] - sa[..., :, None] * b_t[..., None, :]
                 + v_t[..., :, None] * k_t[..., None, :])
        return state, jnp.einsum('dbhvk,dbhk->dbhv', state, r_t)

    state0 = jnp.zeros((2, bsz, B_HEADS, B_HEAD_DIM, B_HEAD_DIM), f32)
    _, out = lax.scan(step, state0, (shared(r_h), per_dir(heads(decay)), per_dir(k_dir),
                                     shared(v_h), shared(kk), per_dir(kk_a)))
    out = jnp.moveaxis(out, 0, 2)
    o = out[0] + jnp.flip(out[1], axis=1)
    mu = jnp.mean(o, -1, keepdims=True)
    var = jnp.mean(jnp.square(o - mu), -1, keepdims=True)
    o = ((o - mu) * lax.rsqrt(var + GN_EPS)).reshape(bsz, seq, B_WIDTH) * gn_g + gn_b
    bonus = jnp.einsum('bshn,bsdhn->bsh', r_h * heads(r_k), k_dir)[..., None] * v_h
    return (o + bonus.reshape(bsz, seq, B_WIDTH)) * g


def t5_bucket(rel):
    nb = REL_BUCKETS // 2
    max_exact = nb // 2
    n = np.abs(rel)
    large = max_exact + (np.log(np.maximum(n, 1) / max_exact) / np.log(REL_MAX_DISTANCE / max_exact)
                         * (nb - max_exact)).astype(np.int32)
    large = np.minimum(large, nb - 1)
    return (rel > 0).astype(np.int32) * nb + np.where(n < max_exact, n, large)


def dilated_group(q, k, v, bias, delta, dilation, half):
    bsz, seq, h, dh = q.shape
    n = seq // dilation
    nb = -(-n // half)
    npad = nb * half

    def strided(t, lo, hi):
        t = t.reshape(bsz, n, dilation, h, dh)
        return jnp.pad(t, ((0, 0), (lo, hi), (0, 0), (0, 0), (0, 0)))

    qs = strided(q, 0, npad - n).reshape(bsz, nb, half, dilation, h, dh)

    def band(t):
        tp = strided(t, half, npad - n + half).reshape(bsz, nb + 2, half, dilation, h, dh)
        return jnp.concatenate([tp[:, :-2], tp[:, 1:-1], tp[:, 2:]], axis=2)

    kb, vb = band(k), band(v)
    key_m = jnp.arange(nb)[:, None] * half - half + jnp.arange(3 * half)[None, :]
    mask = (np.abs(delta) <= half)[None] & ((key_m >= 0) & (key_m < n))[:, None, :]
    s = jnp.einsum('bnqrhd,bnkrhd->bnrhqk', qs, kb) * dh ** -0.5 + bias
    s = jnp.where(mask[None, :, None, None], s, NEG_INF)
    lse = jax.nn.logsumexp(s, axis=-1)
    p = jnp.exp(s - lse[..., None])
    o = jnp.einsum('bnrhqk,bnkrhd->bnqrhd', p, vb)
    o = o.reshape(bsz, npad, dilation, h, dh)[:, :n].reshape(bsz, seq, h, dh)
    lse = jnp.moveaxis(lse, -1, 2).reshape(bsz, npad, dilation, h)[:, :n].reshape(bsz, seq, h)
    return o, lse


def dilated_attention(q, k, v, rel_bias):
    outs, lses = [], []
    for gi, (window, dilation) in enumerate(C_PATTERNS):
        half = window // (2 * dilation)
        delta = np.arange(3 * half)[None, :] - half - np.arange(half)[:, None]
        hs = slice(gi * C_HEADS_PER_GROUP, (gi + 1) * C_HEADS_PER_GROUP)
        bias = jnp.moveaxis(rel_bias[t5_bucket(delta * dilation)][..., hs], -1, 0).astype(jnp.float32)
        o, lse = dilated_group(q[:, :, hs], k[:, :, hs], v[:, :, hs], bias, delta, dilation, half)
        outs.append(o)
        lses.append(lse)
    wts = jax.nn.softmax(jnp.stack(lses), axis=0)[..., None]
    o = jnp.sum(jnp.stack(outs) * wts, axis=0)
    return o.reshape(o.shape[0], o.shape[1], C_OUT)


def hierarchical_moe(x, w_rg, b_rg, w_re, b_re, w_gate, w_up, w_down):
    f32 = jnp.float32
    bsz, seq, d = x.shape
    xf = x.reshape(-1, d)
    n_tok = xf.shape[0]
    xr = xf.astype(f32)
    grp_prob = jax.nn.softmax(xr @ w_rg.astype(f32) + b_rg.astype(f32), axis=-1)
    grp_w, grp = lax.top_k(grp_prob, 1)
    e_logits = (xr @ w_re.astype(f32) + b_re.astype(f32)).reshape(n_tok, N_GROUPS, EXPERTS_PER_GROUP)
    in_grp = jnp.take_along_axis(e_logits, grp[:, :, None], axis=1)[:, 0]
    top_l, top_i = lax.top_k(in_grp, TOP_K)
    gate = jax.nn.softmax(top_l, axis=-1) * grp_w
    expert = grp * EXPERTS_PER_GROUP + top_i
    flat_e = expert.reshape(-1)
    n_asg = flat_e.shape[0]
    order = jnp.argsort(flat_e)
    e_sorted = flat_e[order]
    tok_sorted = (order // TOP_K).astype(jnp.int32)
    gate_sorted = gate.reshape(-1)[order]
    counts = jnp.bincount(flat_e, length=N_EXPERTS)
    padded = (counts + MOE_BLOCK - 1) // MOE_BLOCK * MOE_BLOCK
    pad_end = jnp.cumsum(padded)
    pad_start = pad_end - padded
    start = jnp.cumsum(counts) - counts
    dest = pad_start[e_sorted] + jnp.arange(n_asg) - start[e_sorted]
    n_rows = (n_asg + MOE_BLOCK - 1) // MOE_BLOCK * MOE_BLOCK + N_EXPERTS * MOE_BLOCK
    n_blk = n_rows // MOE_BLOCK
    row_tok = jnp.full((n_rows,), n_tok, jnp.int32).at[dest].set(tok_sorted)
    blk_expert = jnp.minimum(jnp.searchsorted(pad_end, jnp.arange(n_blk) * MOE_BLOCK, side='right'),
                             N_EXPERTS - 1)
    x_pad = jnp.concatenate([xf, jnp.zeros((1, d), xf.dtype)], axis=0)

    def expert_block(args):
        toks, e = args
        xb = x_pad[toks]
        hid = jax.nn.silu(xb @ w_gate[e]) * (xb @ w_up[e])
        return hid @ w_down[e]

    y_rows = lax.map(expert_block, (row_tok.reshape(n_blk, MOE_BLOCK), blk_expert)).reshape(n_rows, d)
    y = jnp.zeros((n_tok, d), f32).at[tok_sorted].add(y_rows[dest].astype(f32) * gate_sorted[:, None])
    return y.reshape(bsz, seq, d).astype(x.dtype)


def setup_inputs(seed: int = 0) -> dict:
    key = jax.random.key(seed)
    ks = iter(jax.random.split(key, 40))
    f32 = jnp.float32
    L, D = DEPTH, D_MODEL
    beta = (8.0 * DEPTH) ** -0.25

    def nrm(shape, scale):
        return jax.random.normal(next(ks), shape, f32) * scale

    def uni(shape, lo, hi):
        return jax.random.uniform(next(ks), shape, f32, lo, hi)

    return {
        'x': nrm((BATCH, SEQ, D), 1.0),
        'w_in': nrm((L, D, IN_COLS), D ** -0.5),
        'w_branch': nrm((L, BRANCH_ROWS, D), A_OUT ** -0.5),
        'w_out': nrm((L, D, D), beta * D ** -0.5),
        'mu_prev': uni((L, B_COLS), 0.0, 0.5),
        'mu_next': uni((L, B_COLS), 0.0, 0.5),
        'rwkv_w0': uni((L, 2, B_WIDTH), -5.0, 0.0),
        'rwkv_w2': nrm((L, 2, DECAY_LORA, B_WIDTH), 0.5 * DECAY_LORA ** -0.5),
        'rwkv_a0': nrm((L, 2, B_WIDTH), 0.5),
        'rwkv_a2': nrm((L, 2, ICLR_LORA, B_WIDTH), 0.5 * ICLR_LORA ** -0.5),
        'rwkv_g2': nrm((L, GATE_LORA, B_WIDTH), GATE_LORA ** -0.5),
        'rwkv_k_k': 0.85 + nrm((L, B_WIDTH), 0.02),
        'rwkv_k_a': 1.0 + nrm((L, B_WIDTH), 0.02),
        'rwkv_r_k': nrm((L, B_WIDTH), 0.1),
        'rwkv_gn_g': 1.0 + nrm((L, B_WIDTH), 0.02),
        'rwkv_gn_b': nrm((L, B_WIDTH), 0.01),
        'q_norm': 1.0 + nrm((L, A_HEAD_DIM), 0.02),
        'k_norm': 1.0 + nrm((L, A_HEAD_DIM), 0.02),
        'rel_bias': nrm((REL_BUCKETS, C_HEADS), 0.5),
        'ln1_g': 1.0 + nrm((L, D), 0.02),
        'ln1_b': nrm((L, D), 0.01),
        'router_group_w': nrm((L, D, N_GROUPS), D ** -0.5),
        'router_group_b': nrm((L, N_GROUPS), 0.01),
        'router_expert_w': nrm((L, D, N_EXPERTS), D ** -0.5),
        'router_expert_b': nrm((L, N_EXPERTS), 0.01),
        'w_gate': nrm((L, N_EXPERTS, D, EXPERT_FF), D ** -0.5),
        'w_up': nrm((L, N_EXPERTS, D, EXPERT_FF), D ** -0.5),
        'w_down': nrm((L, N_EXPERTS, EXPERT_FF, D), beta * EXPERT_FF ** -0.5),
        'ln2_g': 1.0 + nrm((L, D), 0.02),
        'ln2_b': nrm((L, D), 0.01),
    }


def reference(x, w_in, w_branch, w_out, mu_prev, mu_next, rwkv_w0, rwkv_w2, rwkv_a0, rwkv_a2,
              rwkv_g2, rwkv_k_k, rwkv_k_a, rwkv_r_k, rwkv_gn_g, rwkv_gn_b, q_norm, k_norm, rel_bias,
              ln1_g, ln1_b, router_group_w, router_group_b, router_expert_w, router_expert_b,
              w_gate, w_up, w_down, ln2_g, ln2_b):
    f32 = jnp.float32
    alpha = (2.0 * DEPTH) ** 0.25
    bsz, seq, _ = x.shape
    for l in range(DEPTH):
        proj = jnp.einsum('bsd,dc->bsc', x, w_in[l])
        p_a, p_b, p_c, p_g = _split(proj, [A_COLS, B_COLS, C_COLS, GATE_COLS])
        qa, ka, va = _split(p_a, [A_Q, A_KV, A_KV])
        y_a = gqa_axial_attention(qa.reshape(bsz, seq, A_HEADS, A_HEAD_DIM),
                                  ka.reshape(bsz, seq, A_KV_HEADS, A_HEAD_DIM),
                                  va.reshape(bsz, seq, A_KV_HEADS, A_HEAD_DIM), q_norm[l], k_norm[l])
        y_b = rwkv7_bidirectional(p_b, mu_prev[l], mu_next[l], rwkv_w0[l], rwkv_w2[l], rwkv_a0[l],
                                  rwkv_a2[l], rwkv_g2[l], rwkv_k_k[l], rwkv_k_a[l], rwkv_r_k[l],
                                  rwkv_gn_g[l], rwkv_gn_b[l])
        qc, kc, vc = (t.reshape(bsz, seq, C_HEADS, C_HEAD_DIM).astype(f32) for t in _split(p_c, [C_QKV] * 3))
        y_c = dilated_attention(qc, kc, vc, rel_bias)
        gates = jax.nn.sigmoid(p_g.astype(f32)).reshape(bsz, seq, N_BRANCHES, D_MODEL)
        wb_a, wb_b, wb_c = jnp.split(w_branch[l], [A_OUT, A_OUT + B_OUT], axis=0)
        merged = (gates[:, :, 0] * (y_a.astype(x.dtype) @ wb_a)
                  + gates[:, :, 1] * (y_b.astype(x.dtype) @ wb_b)
                  + gates[:, :, 2] * (y_c.astype(x.dtype) @ wb_c))
        x = layer_norm(alpha * x + merged.astype(x.dtype) @ w_out[l], ln1_g[l], ln1_b[l])
        moe = hierarchical_moe(x, router_group_w[l], router_group_b[l], router_expert_w[l],
                               router_expert_b[l], w_gate[l], w_up[l], w_down[l])
        x = layer_norm(alpha * x + moe, ln2_g[l], ln2_b[l])
    return x
```

```python
import numpy as np
from contextlib import ExitStack
import concourse.bass as bass
import concourse.mybir as mybir

F32 = mybir.dt.float32
BF16 = mybir.dt.bfloat16
I32 = mybir.dt.int32
U32 = mybir.dt.uint32
AF = mybir.ActivationFunctionType
ALU = mybir.AluOpType
AX = mybir.AxisListType

SAME_ENGINE_SYNC = True


class Buf:
    def __init__(self, S, t, name, dram=False):
        self.S = S
        self.t = t
        self.name = name
        self.dram = dram
        self.w = {}
        self.r = {}
        self.dsem = None
        self.dcount = 0

    def __getitem__(self, idx):
        return self.t[idx]

    def ap(self):
        return self.t.ap() if self.dram else self.t[:]


class Sched:
    def __init__(self, nc, es):
        self.nc = nc
        self.es = es
        self.eng = {'pe': nc.tensor, 'dve': nc.vector, 'act': nc.scalar, 'pool': nc.gpsimd, 'sp': nc.sync}
        self.sem = {}
        self.cnt = {}
        self.known = {}
        for e in self.eng:
            self.sem[e] = es.enter_context(nc.semaphore('s_' + e))
            self.cnt[e] = 0
            self.known[e] = {}
        self.dma_owner = {}
        self.nbuf = 0
        self.ninst = 0
        self.stacks = [es]
        self.free_dsems = []

    def scope(self):
        S = self

        class _Scope:
            def __enter__(s2):
                s2.st = ExitStack()
                s2.st.__enter__()
                S.stacks.append(s2.st)
                s2.owners0 = set(S.dma_owner.keys())
                return s2

            def __exit__(s2, *a):
                S.barrier()
                for k in list(S.dma_owner.keys()):
                    if k not in s2.owners0:
                        b = S.dma_owner.pop(k)
                        S.free_dsems.append((b.dsem, b.dcount))
                S.stacks.pop()
                return s2.st.__exit__(*a)
        return _Scope()

    def barrier(self):
        toks = {}
        for e in self.eng:
            if self.cnt[e] > 0:
                toks[id(self.sem[e])] = (self.sem[e], self.cnt[e])
        for sid, b in self.dma_owner.items():
            if b.dcount > 0:
                toks[sid] = (b.dsem, b.dcount)
        for e in self.eng:
            self._need(e, toks)

    def sb(self, name, shape, dt=F32):
        self.nbuf += 1
        name = "%s_%d" % (name, self.nbuf)
        t = self.stacks[-1].enter_context(self.nc.sbuf_tensor(name, list(shape), dt))
        return Buf(self, t, name)

    def ps(self, name, shape, dt=F32):
        t = self.es.enter_context(self.nc.psum_tensor(name, list(shape), dt))
        return Buf(self, t, name)

    def dram(self, name, shape, dt=F32, kind='Internal'):
        t = self.nc.dram_tensor(name, list(shape), dt, kind=kind)
        return Buf(self, t, name, dram=True)

    def _need(self, e, toks):
        own = id(self.sem[e]) if e in self.sem else None
        for sid, (sem, val) in toks.items():
            if sid == own and (not SAME_ENGINE_SYNC or e == 'pe'):
                continue
            k = self.known[e].get(sid, 0)
            if k >= val:
                continue
            owner = self.dma_owner.get(sid)
            if owner is not None:
                val = owner.dcount
            self.eng[e].wait_ge(sem, val)
            self.known[e][sid] = val

    @staticmethod
    def _merge(dst, toks):
        for sid, (sem, val) in toks.items():
            if sid not in dst or dst[sid][1] < val:
                dst[sid] = (sem, val)

    def _deps(self, e, reads, writes):
        toks = {}
        for b in reads:
            self._merge(toks, b.w)
        for b in writes:
            self._merge(toks, b.w)
            self._merge(toks, b.r)
        self._need(e, toks)

    def _commit(self, tok, reads, writes):
        sid = id(tok[0])
        for b in reads:
            if sid not in b.r or b.r[sid][1] < tok[1]:
                b.r[sid] = tok
        for b in writes:
            b.r = {}
            if sid not in b.w or b.w[sid][1] < tok[1]:
                b.w[sid] = tok

    def op(self, e, fn, reads=(), writes=()):
        self._deps(e, reads, writes)
        inst = fn(self.eng[e])
        self.cnt[e] += 1
        inst.then_inc(self.sem[e], 1)
        self.ninst += 1
        self._commit((self.sem[e], self.cnt[e]), reads, writes)
        return inst

    def dma(self, q, out_ap, in_ap, reads=(), writes=(), sbuf=None, **kw):
        if sbuf is None:
            for b in list(writes) + list(reads):
                if not b.dram:
                    sbuf = b
                    break
        if sbuf is None:
            sbuf = writes[0]
        if sbuf.dsem is None:
            if self.free_dsems:
                sbuf.dsem, sbuf.dcount = self.free_dsems.pop()
            else:
                sbuf.dsem = self.es.enter_context(self.nc.semaphore('d%d' % len(self.dma_owner) + sbuf.name))
            self.dma_owner[id(sbuf.dsem)] = sbuf
        self._deps(q, reads, writes)
        inst = self.eng[q].dma_start(out=out_ap, in_=in_ap, **kw)
        sbuf.dcount += 16
        inst.then_inc(sbuf.dsem, 16)
        self.ninst += 1
        self._commit((sbuf.dsem, sbuf.dcount), reads, writes)
        return inst

    def finish(self, bufs, e='sp'):
        toks = {}
        for b in bufs:
            self._merge(toks, b.w)
        self._need(e, toks)


def rope_tables(SEQ):
    pos = np.arange(SEQ)
    row = (pos // 64).astype(np.float32); col = (pos % 64).astype(np.float32)
    inv = (10000.0 ** (-np.arange(0, 64, 2, dtype=np.float32) / 64)).astype(np.float32)
    ar = row[:, None] * inv[None, :]; ac = col[:, None] * inv[None, :]
    cr, sr, cc, sc = np.cos(ar), np.sin(ar), np.cos(ac), np.sin(ac)
    COS = np.concatenate([cr, cr, cc, cc], 1).astype(np.float32)
    SINS = np.concatenate([-sr, sr, -sc, sc], 1).astype(np.float32)
    return COS, SINS
def t5_bucket(rel):
    nb = 16; max_exact = 8
    n = np.abs(rel)
    large = max_exact + (np.log(np.maximum(n, 1) / max_exact) / np.log(1024 / max_exact) * (nb - max_exact)).astype(np.int32)
    large = np.minimum(large, nb - 1)
    return (rel > 0).astype(np.int32) * nb + np.where(n < max_exact, n, large)
def bias_tables(rel_bias):
    out = np.full((12, 2, 128, 256), -30000.0, np.float32)
    p = np.arange(128)[:, None]; f = np.arange(256)[None, :]
    delta = 64 + p - f
    mask = np.abs(delta) <= 64
    for g, d in enumerate((1, 4, 16)):
        b = t5_bucket(delta * d)
        for h in range(4):
            hh = g * 4 + h
            vals = rel_bias[b, hh]
            out[hh, 0] = np.where(mask, vals, -30000.0)
            out[hh, 1, 0:64] = out[hh, 0, 64:128]
    return out
def sel_table():
    s = np.zeros((128, 64, 128), np.float32)
    for h2 in range(2):
        for t in range(64):
            s[h2 * 64 + t, t, h2 * 64:(h2 + 1) * 64] = 1.0
    return s
def anti_ident():
    return np.ascontiguousarray(np.eye(128, dtype=np.float32)[::-1])


D = 2048
A_Q, A_KV = 1024, 256
A_COLS = 1536
B_COLS = 3456
C_QKV = 1536
C_COLS = 4608
G_COLS = 6144
IN_COLS = 15744
OFF_B = A_COLS
OFF_C = A_COLS + B_COLS
OFF_G = OFF_C + C_COLS
NEXP = 64
FF = 384
ALPHA = 8.0 ** 0.25
C_DIL = (1, 4, 16)


def common(S_, nc):
    pass


def phase_proj(S, x_d, w_d, proj_d, ident, PS, SEQ):
    KC = D // 128
    TG = 1024 if SEQ % 1024 == 0 else 512
    with S.scope():
        xT = S.sb("p1_xT", [128, KC, TG])
        xin = [S.sb(f"p1_xin{i}", [128, D]) for i in range(2)]
        wt = [S.sb(f"p1_wt{i}", [128, KC, 512]) for i in range(2)]
        ot = [S.sb(f"p1_ot{i}", [128, 512]) for i in range(4)]
        wv = w_d.ap().rearrange("(k p) c -> p k c", p=128)
        nblk = (IN_COLS + 511) // 512
        cnt = 0
        wcnt = 0
        for tg in range(SEQ // TG):
            for m in range(TG // 128):
                xi = xin[m % 2]
                r0 = tg * TG + m * 128
                S.dma('sp', xi[:], x_d.ap()[r0:r0 + 128, :], reads=[x_d], writes=[xi])
                for kg in range(KC // 4):
                    p = PS[6 + kg % 2]
                    for j in range(4):
                        k = kg * 4 + j
                        S.op('pe', lambda e: e.transpose(p[:, j * 128:(j + 1) * 128], xi[:, k * 128:(k + 1) * 128], ident[:]), reads=[xi, ident], writes=[p])
                    S.op('dve', lambda e: e.tensor_copy(xT[:, kg * 4:kg * 4 + 4, m * 128:(m + 1) * 128], p[:].rearrange("p (j t) -> p j t", j=4)), reads=[p], writes=[xT])
            for n in range(nblk):
                c0 = n * 512
                cw = min(512, IN_COLS - c0)
                wb = wt[wcnt % 2]
                wcnt += 1
                S.dma('act' if n % 2 else 'sp', wb[:, :, 0:cw], wv[:, :, c0:c0 + cw], reads=[w_d], writes=[wb])
                for m in range(TG // 128):
                    a = PS[cnt % 4]
                    o = ot[cnt % 4]
                    cnt += 1
                    for k in range(KC):
                        S.op('pe', lambda e: e.matmul(a[:, 0:cw], xT[:, k, m * 128:(m + 1) * 128], wb[:, k, 0:cw], start=(k == 0), stop=(k == KC - 1)), reads=[xT, wb], writes=[a])
                    if cnt % 2:
                        S.op('act', lambda e: e.copy(o[:, 0:cw], a[:, 0:cw]), reads=[a], writes=[o])
                    else:
                        S.op('dve', lambda e: e.tensor_copy(o[:, 0:cw], a[:, 0:cw]), reads=[a], writes=[o])
                    r0 = tg * TG + m * 128
                    S.dma('sp', proj_d.ap()[r0:r0 + 128, c0:c0 + cw], o[:, 0:cw], reads=[o], writes=[proj_d])


def phase_attn_a(S, proj_d, ya_d, qkn_d, cos_d, sin_d, qg_ap, kg_ap, ident, PS, SEQ):
    NT = SEQ // 128
    with S.scope():
        gq = S.sb("a_gq", [128, 128])
        gk = S.sb("a_gk", [128, 128])
        S.dma('sp', gq[:], qg_ap.partition_broadcast(128), reads=[], writes=[gq])
        S.dma('sp', gk[:], kg_ap.partition_broadcast(128), reads=[], writes=[gk])
        tin = [S.sb(f"a_tin{i}", [128, 1280]) for i in range(2)]
        cs = [S.sb(f"a_cs{i}", [128, 2, 128]) for i in range(2)]
        sq = S.sb("a_sq", [128, 1280])
        tmp = S.sb("a_tmp", [128, 1280])
        ss = S.sb("a_ss", [128, 10])
        for i in range(NT):
            t = tin[i % 2]
            c = cs[i % 2]
            r0 = i * 128
            S.dma('sp', t[:], proj_d.ap()[r0:r0 + 128, 0:1280], reads=[proj_d], writes=[t])
            S.dma('act', c[:, 0, :], cos_d.ap()[r0:r0 + 128, :], reads=[cos_d], writes=[c])
            S.dma('act', c[:, 1, :], sin_d.ap()[r0:r0 + 128, :], reads=[sin_d], writes=[c])
            S.op('dve', lambda e: e.tensor_tensor(out=sq[:], in0=t[:], in1=t[:], op=ALU.mult), reads=[t], writes=[sq])
            S.op('dve', lambda e: e.tensor_reduce(out=ss[:], in_=sq[:].rearrange("p (h d) -> p h d", h=10), axis=AX.X, op=ALU.add), reads=[sq], writes=[ss])
            S.op('dve', lambda e: e.tensor_scalar(out=ss[:], in0=ss[:], scalar1=1.0 / 128, scalar2=1e-6, op0=ALU.mult, op1=ALU.add), reads=[ss], writes=[ss])
            S.op('act', lambda e: e.activation(out=ss[:], in_=ss[:], func=AF.Sqrt), reads=[ss], writes=[ss])
            S.op('dve', lambda e: e.reciprocal(out=ss[:], in_=ss[:]), reads=[ss], writes=[ss])
            for h in range(10):
                g = gq if h < 8 else gk
                S.op('dve', lambda e: e.scalar_tensor_tensor(out=t[:, h * 128:(h + 1) * 128], in0=t[:, h * 128:(h + 1) * 128], scalar=ss[:, h:h + 1], in1=g[:], op0=ALU.mult, op1=ALU.mult), reads=[t, ss, g], writes=[t])
            tv = t[:].rearrange("p (h a b f) -> p h a b f", h=10, a=2, b=2)
            mv = tmp[:].rearrange("p (h a b f) -> p h a b f", h=10, a=2, b=2)
            sv = c[:, 1, :].rearrange("p (a b f) -> p a b f", a=2, b=2)
            for b in range(2):
                S.op('dve', lambda e: e.tensor_tensor(out=mv[:, :, :, b, :], in0=tv[:, :, :, 1 - b, :], in1=sv[:, :, b, :].unsqueeze(1).broadcast_to([128, 10, 2, 32]), op=ALU.mult), reads=[t, c], writes=[tmp])
            S.op('dve', lambda e: e.tensor_tensor(out=t[:].rearrange("p (h d) -> p h d", h=10), in0=t[:].rearrange("p (h d) -> p h d", h=10), in1=c[:, 0, :].unsqueeze(1).broadcast_to([128, 10, 128]), op=ALU.mult), reads=[t, c], writes=[t])
            S.op('dve', lambda e: e.tensor_tensor(out=t[:], in0=t[:], in1=tmp[:], op=ALU.add), reads=[t, tmp], writes=[t])
            S.dma('sp', qkn_d.ap()[r0:r0 + 128, :], t[:], reads=[t], writes=[qkn_d])
    with S.scope():
        kT = S.sb("a_kT", [128, SEQ])
        qT = S.sb("a_qT", [128, SEQ])
        V = S.sb("a_V", [128, NT, 129])
        tl = [S.sb(f"a_tl{i}", [128, 128]) for i in range(4)]
        pT = [S.sb(f"a_pT{i}", [128, 512]) for i in range(2)]
        o_sb = [S.sb(f"a_o{i}", [128, 128]) for i in range(2)]
        rec = S.sb("a_rec", [128, 1])
        S.op('dve', lambda e: e.memset(V[:], 1.0), writes=[V])
        tcnt = 0

        def load_T(dst, col0, src_d):
            nonlocal tcnt
            for i in range(NT):
                tt = tl[tcnt % 4]
                p = PS[6 + tcnt % 2]
                tcnt += 1
                S.dma('sp', tt[:], src_d.ap()[i * 128:(i + 1) * 128, col0:col0 + 128], reads=[src_d], writes=[tt])
                S.op('pe', lambda e: e.transpose(p[:, 0:128], tt[:], ident[:]), reads=[tt, ident], writes=[p])
                S.op('dve', lambda e: e.tensor_copy(dst[:, i * 128:(i + 1) * 128], p[:, 0:128]), reads=[p], writes=[dst])

        ocnt = 0
        for kvh in range(2):
            load_T(kT, 1024 + kvh * 128, qkn_d)
            S.dma('sp', V[:, :, 0:128], proj_d.ap()[:, 1280 + kvh * 128:1280 + (kvh + 1) * 128].rearrange("(t p) d -> p t d", p=128), reads=[proj_d], writes=[V])
            for g in range(4):
                h = kvh * 4 + g
                load_T(qT, h * 128, qkn_d)
                for qb in range(SEQ // 512):
                    for j in range(NT):
                        sp_ = PS[4 + j % 2]
                        pt = pT[j % 2]
                        S.op('pe', lambda e: e.matmul(sp_[:], kT[:, j * 128:(j + 1) * 128], qT[:, qb * 512:(qb + 1) * 512], start=True, stop=True), reads=[kT, qT], writes=[sp_])
                        S.op('act', lambda e: e.activation(out=pt[:], in_=sp_[:], func=AF.Exp, scale=128.0 ** -0.5), reads=[sp_], writes=[pt])
                        for sub in range(4):
                            S.op('pe', lambda e: e.matmul(PS[sub][:, 0:129], pt[:, sub * 128:(sub + 1) * 128], V[:, j, :], start=(j == 0), stop=(j == NT - 1)), reads=[pt, V], writes=[PS[sub]])
                    for sub in range(4):
                        o = o_sb[ocnt % 2]
                        ocnt += 1
                        S.op('dve', lambda e: e.reciprocal(out=rec[:], in_=PS[sub][:, 128:129]), reads=[PS[sub]], writes=[rec])
                        S.op('dve', lambda e: e.tensor_scalar(out=o[:], in0=PS[sub][:, 0:128], scalar1=rec[:, 0:1], scalar2=None, op0=ALU.mult), reads=[PS[sub], rec], writes=[o])
                        r0 = qb * 512 + sub * 128
                        S.dma('sp', ya_d.ap()[r0:r0 + 128, h * 128:(h + 1) * 128], o[:], reads=[o], writes=[ya_d])


def bcast_row(S, name, ap_row, n, q='sp'):
    t = S.sb(name, [128, n])
    S.dma(q, t[:], ap_row.partition_broadcast(128), reads=[], writes=[t])
    return t


def layer_norm(S, x, xs, g_bc, b_bc, out, outs, scr, st, eps=1e-5):
    S.op('dve', lambda e: e.tensor_reduce(out=st[:, 0:1], in_=xs, axis=AX.X, op=ALU.add), reads=[x], writes=[st])
    S.op('dve', lambda e: e.tensor_scalar(out=st[:, 0:1], in0=st[:, 0:1], scalar1=-1.0 / D, scalar2=None, op0=ALU.mult), reads=[st], writes=[st])
    S.op('dve', lambda e: e.tensor_scalar(out=scr[:], in0=xs, scalar1=st[:, 0:1], scalar2=None, op0=ALU.add), reads=[x, st], writes=[scr])
    S.op('dve', lambda e: e.tensor_tensor(out=outs, in0=scr[:], in1=scr[:], op=ALU.mult), reads=[scr], writes=[out])
    S.op('dve', lambda e: e.tensor_reduce(out=st[:, 1:2], in_=outs, axis=AX.X, op=ALU.add), reads=[out], writes=[st])
    S.op('dve', lambda e: e.tensor_scalar(out=st[:, 1:2], in0=st[:, 1:2], scalar1=1.0 / D, scalar2=eps, op0=ALU.mult, op1=ALU.add), reads=[st], writes=[st])
    S.op('act', lambda e: e.activation(out=st[:, 1:2], in_=st[:, 1:2], func=AF.Sqrt), reads=[st], writes=[st])
    S.op('dve', lambda e: e.reciprocal(out=st[:, 1:2], in_=st[:, 1:2]), reads=[st], writes=[st])
    S.op('dve', lambda e: e.scalar_tensor_tensor(out=outs, in0=scr[:], scalar=st[:, 1:2], in1=g_bc[:], op0=ALU.mult, op1=ALU.mult), reads=[scr, st, g_bc], writes=[out])
    S.op('dve', lambda e: e.tensor_tensor(out=outs, in0=outs, in1=b_bc[:], op=ALU.add), reads=[out, b_bc], writes=[out])


def phase_merge(S, ya_d, yb_d, yc_d, proj_d, x_d, wb_ap, wo_ap, g1_ap, b1_ap, wr_ap, rb_ap,
                x1_d, x1T_d, G_d, ident, PS, SEQ, NGRP):
    TG = 256
    NSUB = 2
    NE = NGRP * 8
    NR = NGRP + NE
    with S.scope():
        g_bc = bcast_row(S, "m_g", g1_ap, D)
        b_bc = bcast_row(S, "m_b", b1_ap, D)
        rb_bc = bcast_row(S, "m_rb", rb_ap, NR)
        wr = S.sb("m_wr", [128, 16, NR])
        S.dma('sp', wr[:], wr_ap.rearrange("(p k) c -> p k c", k=16), reads=[], writes=[wr])
        yT = S.sb("m_yT", [128, 20, TG])
        yin = [S.sb(f"m_yin{i}", [128, 2560]) for i in range(2)]
        merged = S.sb("m_merged", [128, NSUB, D])
        mT = S.sb("m_mT", [128, 16, TG])
        wblk = [S.sb(f"m_w{i}", [128, 16, 512]) for i in range(2)]
        gt = [S.sb(f"m_gt{i}", [128, 512]) for i in range(2)]
        tmp = S.sb("m_tmp", [128, 512])
        scr = S.sb("m_scr", [128, D])
        x1 = S.sb("m_x1", [128, D])
        x1T = S.sb("m_x1T", [128, 16, 128])
        st = S.sb("m_st", [128, 4])
        lg = S.sb("m_lg", [128, NR])
        r8 = S.sb("m_r8", [128, 8 * 8])
        sm = S.sb("m_sm", [128, 16])
        oh = S.sb("m_oh", [128, 3, 8])
        mx8 = S.sb("m_mx8", [128, 8])
        sel = S.sb("m_sel", [128, 8])
        gin = S.sb("m_gin", [128, 8])
        G = S.sb("m_G", [128, NE])
        wcnt = 0
        gcnt = 0
        pcnt = 0
        for tg in range(SEQ // TG):
            t0 = tg * TG
            for sub in range(NSUB):
                yi = yin[sub % 2]
                r0 = t0 + sub * 128
                S.dma('sp', yi[:, 0:1024], ya_d.ap()[r0:r0 + 128, :], reads=[ya_d], writes=[yi])
                S.dma('act', yi[:, 1024:2048], yb_d.ap()[r0:r0 + 128, :], reads=[yb_d], writes=[yi])
                S.dma('sp', yi[:, 2048:2560], yc_d.ap()[r0:r0 + 128, :], reads=[yc_d], writes=[yi])
                for kg in range(5):
                    p = PS[6 + kg % 2]
                    for j in range(4):
                        k = kg * 4 + j
                        S.op('pe', lambda e: e.transpose(p[:, j * 128:(j + 1) * 128], yi[:, k * 128:(k + 1) * 128], ident[:]), reads=[yi, ident], writes=[p])
                    S.op('dve', lambda e: e.tensor_copy(yT[:, kg * 4:kg * 4 + 4, sub * 128:(sub + 1) * 128], p[:].rearrange("p (j t) -> p j t", j=4)), reads=[p], writes=[yT])
            for n in range(4):
                for br, (koff, kc) in enumerate(((0, 8), (8, 8), (16, 4))):
                    wb = wblk[wcnt % 2]
                    wcnt += 1
                    S.dma('sp', wb[:, 0:kc, :], wb_ap[koff * 128:(koff + kc) * 128, n * 512:(n + 1) * 512].rearrange("(k p) c -> p k c", p=128), reads=[], writes=[wb])
                    for sub in range(NSUB):
                        a = PS[pcnt % 4]
                        pcnt += 1
                        g = gt[gcnt % 2]
                        gcnt += 1
                        r0 = t0 + sub * 128
                        c0 = OFF_G + br * D + n * 512
                        S.dma('act', g[:], proj_d.ap()[r0:r0 + 128, c0:c0 + 512], reads=[proj_d], writes=[g])
                        S.op('act', lambda e: e.activation(out=g[:], in_=g[:], func=AF.Sigmoid), reads=[g], writes=[g])
                        for k in range(kc):
                            S.op('pe', lambda e: e.matmul(a[:], yT[:, koff + k, sub * 128:(sub + 1) * 128], wb[:, k, :], start=(k == 0), stop=(k == kc - 1)), reads=[yT, wb], writes=[a])
                        if br == 0:
                            S.op('dve', lambda e: e.tensor_tensor(out=merged[:, sub, n * 512:(n + 1) * 512], in0=a[:], in1=g[:], op=ALU.mult), reads=[a, g], writes=[merged])
                        else:
                            S.op('dve', lambda e: e.tensor_tensor(out=tmp[:], in0=a[:], in1=g[:], op=ALU.mult), reads=[a, g], writes=[tmp])
                            S.op('dve', lambda e: e.tensor_tensor(out=merged[:, sub, n * 512:(n + 1) * 512], in0=merged[:, sub, n * 512:(n + 1) * 512], in1=tmp[:], op=ALU.add), reads=[merged, tmp], writes=[merged])
            for sub in range(NSUB):
                for kg in range(4):
                    p = PS[6 + kg % 2]
                    for j in range(4):
                        k = kg * 4 + j
                        S.op('pe', lambda e: e.transpose(p[:, j * 128:(j + 1) * 128], merged[:, sub, k * 128:(k + 1) * 128], ident[:]), reads=[merged, ident], writes=[p])
                    S.op('dve', lambda e: e.tensor_copy(mT[:, kg * 4:kg * 4 + 4, sub * 128:(sub + 1) * 128], p[:].rearrange("p (j t) -> p j t", j=4)), reads=[p], writes=[mT])
            for n in range(4):
                wb = wblk[wcnt % 2]
                wcnt += 1
                S.dma('sp', wb[:], wo_ap[:, n * 512:(n + 1) * 512].rearrange("(k p) c -> p k c", p=128), reads=[], writes=[wb])
                for sub in range(NSUB):
                    a = PS[pcnt % 4]
                    pcnt += 1
                    g = gt[gcnt % 2]
                    gcnt += 1
                    r0 = t0 + sub * 128
                    S.dma('act', g[:], x_d.ap()[r0:r0 + 128, n * 512:(n + 1) * 512], reads=[x_d], writes=[g])
                    for k in range(16):
                        S.op('pe', lambda e: e.matmul(a[:], mT[:, k, sub * 128:(sub + 1) * 128], wb[:, k, :], start=(k == 0), stop=(k == 15)), reads=[mT, wb], writes=[a])
                    S.op('dve', lambda e: e.scalar_tensor_tensor(out=merged[:, sub, n * 512:(n + 1) * 512], in0=g[:], scalar=ALPHA, in1=a[:], op0=ALU.mult, op1=ALU.add), reads=[g, a], writes=[merged])
            for sub in range(NSUB):
                r0 = t0 + sub * 128
                layer_norm(S, merged, merged[:, sub, :], g_bc, b_bc, x1, x1[:], scr, st)
                S.dma('sp', x1_d.ap()[r0:r0 + 128, :], x1[:], reads=[x1], writes=[x1_d])
                for kg in range(4):
                    p = PS[6 + kg % 2]
                    for j in range(4):
                        k = kg * 4 + j
                        S.op('pe', lambda e: e.transpose(p[:, j * 128:(j + 1) * 128], x1[:].rearrange("t (p k) -> t k p", k=16)[:, k, :], ident[:]), reads=[x1, ident], writes=[p])
                    S.op('dve', lambda e: e.tensor_copy(x1T[:, kg * 4:kg * 4 + 4, :], p[:].rearrange("p (j t) -> p j t", j=4)), reads=[p], writes=[x1T])
                S.dma('sp', x1T_d.ap()[:, :, r0:r0 + 128].rearrange("k p t -> p k t"), x1T[:], reads=[x1T], writes=[x1T_d])
                a = PS[pcnt % 4]
                pcnt += 1
                for k in range(16):
                    S.op('pe', lambda e: e.matmul(a[:, 0:NR], x1T[:, k, :], wr[:, k, :], start=(k == 0), stop=(k == 15)), reads=[x1T, wr], writes=[a])
                S.op('dve', lambda e: e.tensor_tensor(out=lg[:], in0=a[:, 0:NR], in1=rb_bc[:], op=ALU.add), reads=[a, rb_bc], writes=[lg])
                S.op('dve', lambda e: e.tensor_reduce(out=sm[:, 0:1], in_=lg[:, 0:NGRP], axis=AX.X, op=ALU.max), reads=[lg], writes=[sm])
                S.op('dve', lambda e: e.tensor_scalar(out=sm[:, 1:2], in0=sm[:, 0:1], scalar1=-1.0, scalar2=None, op0=ALU.mult), reads=[sm], writes=[sm])
                S.op('act', lambda e: e.activation(out=oh[:, 2, 0:NGRP], in_=lg[:, 0:NGRP], func=AF.Exp, bias=sm[:, 1:2], scale=1.0), reads=[lg, sm], writes=[oh])
                S.op('dve', lambda e: e.tensor_reduce(out=sm[:, 2:3], in_=oh[:, 2, 0:NGRP], axis=AX.X, op=ALU.add), reads=[oh], writes=[sm])
                S.op('dve', lambda e: e.reciprocal(out=sm[:, 2:3], in_=sm[:, 2:3]), reads=[sm], writes=[sm])
                S.op('dve', lambda e: e.tensor_scalar(out=oh[:, 2, 0:NGRP], in0=lg[:, 0:NGRP], scalar1=sm[:, 0:1], scalar2=None, op0=ALU.is_equal), reads=[lg, sm], writes=[oh])
                lev = lg[:, NGRP:NR].rearrange("p (g e) -> p g e", e=8)
                S.op('dve', lambda e: e.tensor_tensor(out=r8[:, 0:NE].rearrange("p (g e) -> p g e", e=8), in0=lev, in1=oh[:, 2, 0:NGRP].unsqueeze(2).broadcast_to([128, NGRP, 8]), op=ALU.mult), reads=[lg, oh], writes=[r8])
                S.op('dve', lambda e: e.tensor_reduce(out=sel[:], in_=r8[:, 0:NE].rearrange("p (g e) -> p e g", e=8), axis=AX.X, op=ALU.add), reads=[r8], writes=[sel])
                S.op('dve', lambda e: e.max(out=mx8[:], in_=sel[:]), reads=[sel], writes=[mx8])
                S.op('dve', lambda e: e.tensor_scalar(out=oh[:, 0, :], in0=sel[:], scalar1=mx8[:, 0:1], scalar2=None, op0=ALU.is_equal), reads=[sel, mx8], writes=[oh])
                S.op('dve', lambda e: e.tensor_scalar(out=oh[:, 1, :], in0=sel[:], scalar1=mx8[:, 1:2], scalar2=None, op0=ALU.is_equal), reads=[sel, mx8], writes=[oh])
                S.op('dve', lambda e: e.tensor_tensor(out=sm[:, 3:4], in0=mx8[:, 1:2], in1=mx8[:, 0:1], op=ALU.subtract), reads=[mx8], writes=[sm])
                S.op('act', lambda e: e.activation(out=sm[:, 4:5], in_=sm[:, 3:4], func=AF.Exp), reads=[sm], writes=[sm])
                S.op('dve', lambda e: e.tensor_scalar(out=sm[:, 5:6], in0=sm[:, 4:5], scalar1=1.0, scalar2=None, op0=ALU.add), reads=[sm], writes=[sm])
                S.op('dve', lambda e: e.reciprocal(out=sm[:, 5:6], in_=sm[:, 5:6]), reads=[sm], writes=[sm])
                S.op('dve', lambda e: e.tensor_tensor(out=sm[:, 6:7], in0=sm[:, 5:6], in1=sm[:, 2:3], op=ALU.mult), reads=[sm], writes=[sm])
                S.op('dve', lambda e: e.tensor_tensor(out=sm[:, 7:8], in0=sm[:, 6:7], in1=sm[:, 4:5], op=ALU.mult), reads=[sm], writes=[sm])
                S.op('dve', lambda e: e.tensor_scalar(out=gin[:], in0=oh[:, 0, :], scalar1=sm[:, 6:7], scalar2=None, op0=ALU.mult), reads=[oh, sm], writes=[gin])
                S.op('dve', lambda e: e.scalar_tensor_tensor(out=gin[:], in0=oh[:, 1, :], scalar=sm[:, 7:8], in1=gin[:], op0=ALU.mult, op1=ALU.add), reads=[oh, sm, gin], writes=[gin])
                S.op('dve', lambda e: e.tensor_tensor(out=G[:].rearrange("p (g e) -> p g e", e=8), in0=oh[:, 2, 0:NGRP].unsqueeze(2).broadcast_to([128, NGRP, 8]), in1=gin[:].unsqueeze(1).broadcast_to([128, NGRP, 8]), op=ALU.mult), reads=[oh, gin], writes=[G])
                S.dma('sp', G_d.ap()[r0:r0 + 128, :], G[:], reads=[G], writes=[G_d])


def phase_moe(S, x1_d, x1T_d, G_d, wg_ap, wu_ap, wd_ap, g2_ap, b2_ap, xo_d, PS, SEQ, NGRP):
    TG = 512
    NE = NGRP * 8
    with S.scope():
        g_bc = bcast_row(S, "e_g", g2_ap, D)
        b_bc = bcast_row(S, "e_b", b2_ap, D)
        xT = S.sb("e_xT", [128, 16, TG])
        yacc = S.sb("e_yacc", [128, 4, D])
        G = S.sb("e_G", [128, 4, NE])
        Wg = S.sb("e_Wg", [128, 16, FF])
        Wu = S.sb("e_Wu", [128, 16, FF])
        Wd = S.sb("e_Wd", [128, 3, D])
        hT = S.sb("e_hT", [128, 3, TG])
        gs = [S.sb(f"e_gs{i}", [128, TG]) for i in range(2)]
        x1t = [S.sb(f"e_x1{i}", [128, D]) for i in range(2)]
        scr = S.sb("e_scr", [128, D])
        st = S.sb("e_st", [128, 4])
        pc = 0
        for tg in range(SEQ // TG):
            t0 = tg * TG
            S.dma('sp', xT[:], x1T_d.ap()[:, :, t0:t0 + TG].rearrange("k p t -> p k t"), reads=[x1T_d], writes=[xT])
            S.dma('sp', G[:], G_d.ap()[t0:t0 + TG, :].rearrange("(s p) e -> p s e", p=128), reads=[G_d], writes=[G])
            S.op('dve', lambda e: e.memset(yacc[:], 0.0), writes=[yacc])
            for ex in range(NE):
                S.dma('sp', Wg[:], wg_ap[ex].rearrange("(p k) f -> p k f", k=16), reads=[], writes=[Wg])
                S.dma('act', Wu[:], wu_ap[ex].rearrange("(p k) f -> p k f", k=16), reads=[], writes=[Wu])
                S.dma('sp', Wd[:], wd_ap[ex].rearrange("(k p) c -> p k c", p=128), reads=[], writes=[Wd])
                for f in range(3):
                    a = PS[pc % 8]
                    pc += 1
                    g = gs[f % 2]
                    for k in range(16):
                        S.op('pe', lambda e: e.matmul(a[:], Wg[:, k, f * 128:(f + 1) * 128], xT[:, k, :], start=(k == 0), stop=(k == 15)), reads=[Wg, xT], writes=[a])
                    S.op('act', lambda e: e.activation(out=g[:], in_=a[:], func=AF.Silu), reads=[a], writes=[g])
                    a2 = PS[pc % 8]
                    pc += 1
                    for k in range(16):
                        S.op('pe', lambda e: e.matmul(a2[:], Wu[:, k, f * 128:(f + 1) * 128], xT[:, k, :], start=(k == 0), stop=(k == 15)), reads=[Wu, xT], writes=[a2])
                    S.op('dve', lambda e: e.tensor_tensor(out=hT[:, f, :], in0=a2[:], in1=g[:], op=ALU.mult), reads=[a2, g], writes=[hT])
                for sub in range(4):
                    for n in range(4):
                        a = PS[pc % 8]
                        pc += 1
                        for f in range(3):
                            S.op('pe', lambda e: e.matmul(a[:], hT[:, f, sub * 128:(sub + 1) * 128], Wd[:, f, n * 512:(n + 1) * 512], start=(f == 0), stop=(f == 2)), reads=[hT, Wd], writes=[a])
                        S.op('dve', lambda e: e.scalar_tensor_tensor(out=yacc[:, sub, n * 512:(n + 1) * 512], in0=a[:], scalar=G[:, sub, ex:ex + 1], in1=yacc[:, sub, n * 512:(n + 1) * 512], op0=ALU.mult, op1=ALU.add), reads=[a, G, yacc], writes=[yacc])
            for sub in range(4):
                r0 = t0 + sub * 128
                xi = x1t[sub % 2]
                S.dma('sp', xi[:], x1_d.ap()[r0:r0 + 128, :], reads=[x1_d], writes=[xi])
                S.op('dve', lambda e: e.scalar_tensor_tensor(out=yacc[:, sub, :], in0=xi[:], scalar=ALPHA, in1=yacc[:, sub, :], op0=ALU.mult, op1=ALU.add), reads=[xi, yacc], writes=[yacc])
                layer_norm(S, yacc, yacc[:, sub, :], g_bc, b_bc, xi, xi[:], scr, st)
                S.dma('sp', xo_d.ap()[r0:r0 + 128, :], xi[:], reads=[xi], writes=[xo_d])


def phase_dilated(S, proj_d, nz_d, yc_d, bt_d, ident, PS, SEQ):
    with S.scope():
        qT = S.sb("c_qT", [128, SEQ])
        kT = S.sb("c_kT", [128, SEQ])
        tl = [S.sb(f"c_tl{i}", [128, 128]) for i in range(4)]
        BT = S.sb("c_BT", [128, 2, 256])
        sb_s = [S.sb(f"c_s{i}", [128, 256]) for i in range(2)]
        pT = [S.sb(f"c_pT{i}", [128, 256]) for i in range(2)]
        o_sb = [S.sb(f"c_o{i}", [128, 129]) for i in range(2)]
        tcnt = 0
        scnt = 0
        ocnt = 0
        for g in range(3):
            d = C_DIL[g]
            n = SEQ // d
            nt = n // 128
            V = S.sb(f"c_V{g}", [128, d * (nt + 1), 129])
            S.op('dve', lambda e: e.memset(V[:], 1.0), writes=[V])
            for h in range(4):
                hh = g * 4 + h
                S.dma('sp', BT[:], bt_d.ap()[hh].rearrange("a p f -> p a f"), reads=[bt_d], writes=[BT])
                pv = proj_d.ap().rearrange("(m r) c -> r m c", r=d)
                for r in range(d):
                    for i in range(nt):
                        for (dst, c0) in ((qT, OFF_C + hh * 128), (kT, OFF_C + C_QKV + hh * 128)):
                            tt = tl[tcnt % 4]
                            p = PS[6 + tcnt % 2]
                            tcnt += 1
                            S.dma('sp' if tcnt % 2 else 'act', tt[:], pv[r, i * 128:(i + 1) * 128, c0:c0 + 128], reads=[proj_d], writes=[tt])
                            S.op('pe', lambda e: e.transpose(p[:, 0:128], tt[:], ident[:]), reads=[tt, ident], writes=[p])
                            S.op('dve', lambda e: e.tensor_copy(dst[:, r * n + i * 128:r * n + (i + 1) * 128], p[:, 0:128]), reads=[p], writes=[dst])
                    cv = OFF_C + 2 * C_QKV + hh * 128
                    S.dma('sp', V[0:64, r * (nt + 1), 0:128], pv[r, 0:64, cv:cv + 128], reads=[proj_d], writes=[V])
                    if nt > 1:
                        S.dma('sp', V[:, r * (nt + 1) + 1:r * (nt + 1) + nt, 0:128], pv[r, 64:64 + (nt - 1) * 128, cv:cv + 128].rearrange("(t p) c -> p t c", p=128), reads=[proj_d], writes=[V])
                    S.dma('sp', V[0:64, r * (nt + 1) + nt, 0:128], pv[r, n - 64:n, cv:cv + 128], reads=[proj_d], writes=[V])
                for r in range(d):
                    for mt in range(-1, nt):
                        if mt == -1:
                            k0, npk, bsel, f0, nq, q0 = 0, 64, 1, 128, 128, 0
                        elif mt == nt - 1:
                            k0, npk, bsel, f0, nq, q0 = n - 64, 64, 0, 0, 128, mt * 128
                        else:
                            k0, npk, bsel, f0, nq, q0 = mt * 128 + 64, 128, 0, 0, 256, mt * 128
                        sp_ = PS[4 + scnt % 2]
                        ss_ = sb_s[scnt % 2]
                        pt = pT[scnt % 2]
                        scnt += 1
                        S.op('pe', lambda e: e.matmul(sp_[0:npk, 0:nq], kT[:, r * n + k0:r * n + k0 + npk], qT[:, r * n + q0:r * n + q0 + nq], start=True, stop=True), reads=[kT, qT], writes=[sp_])
                        S.op('dve', lambda e: e.scalar_tensor_tensor(out=ss_[0:npk, 0:nq], in0=sp_[0:npk, 0:nq], scalar=128.0 ** -0.5, in1=BT[0:npk, bsel, f0:f0 + nq], op0=ALU.mult, op1=ALU.add), reads=[sp_, BT], writes=[ss_])
                        S.op('act', lambda e: e.activation(out=pt[0:npk, 0:nq], in_=ss_[0:npk, 0:nq], func=AF.Exp), reads=[ss_], writes=[pt])
                        vt = V[0:npk, r * (nt + 1) + mt + 1, :]
                        for fb in range(nq // 128):
                            qb = q0 // 128 + fb
                            first = (mt == qb - 1)
                            acc = PS[qb % 2]
                            S.op('pe', lambda e: e.matmul(acc[:, 0:129], pt[0:npk, fb * 128:(fb + 1) * 128], vt, start=first, stop=(not first)), reads=[pt, V], writes=[acc])
                            if not first:
                                o = o_sb[ocnt % 2]
                                ocnt += 1
                                S.op('dve', lambda e: e.tensor_copy(o[:], acc[:, 0:129]), reads=[acc], writes=[o])
                                dst = nz_d.ap()[g].rearrange("(m r) h c -> r m h c", r=d)[r, qb * 128:(qb + 1) * 128, h, :]
                                S.dma('sp', dst, o[:], reads=[o], writes=[nz_d])
        nzt = [S.sb(f"c_nz{i}", [128, 3, 4, 129]) for i in range(2)]
        yo = [S.sb(f"c_yo{i}", [128, 4, 128]) for i in range(2)]
        acc4 = S.sb("c_acc4", [128, 4, 129])
        for i in range(SEQ // 128):
            t = nzt[i % 2]
            y = yo[i % 2]
            S.dma('sp', t[:], nz_d.ap()[:, i * 128:(i + 1) * 128, :, :].rearrange("g p h c -> p g h c"), reads=[nz_d], writes=[t])
            S.op('dve', lambda e: e.tensor_tensor(out=acc4[:], in0=t[:, 0], in1=t[:, 1], op=ALU.add), reads=[t], writes=[acc4])
            S.op('dve', lambda e: e.tensor_tensor(out=acc4[:], in0=acc4[:], in1=t[:, 2], op=ALU.add), reads=[t, acc4], writes=[acc4])
            S.op('dve', lambda e: e.reciprocal(out=acc4[:, :, 128:129], in_=acc4[:, :, 128:129]), reads=[acc4], writes=[acc4])
            S.op('dve', lambda e: e.tensor_tensor(out=y[:], in0=acc4[:, :, 0:128], in1=acc4[:, :, 128:129].broadcast_to([128, 4, 128]), op=ALU.mult), reads=[acc4], writes=[y])
            S.dma('sp', yc_d.ap()[i * 128:(i + 1) * 128, :], y[:].rearrange("p h c -> p (h c)"), reads=[y], writes=[yc_d])


def phase_rwkv(S, proj_d, yb_d, Xd, VTd, Od, bon_d, gg_d, prm, sel_d, ident, J, PS, SEQ, parts=(1, 1, 1)):
    NT = SEQ // 128
    NB = 3456
    if parts[0]:
      with S.scope():
        y = S.sb("b_y", [128, NB])
        pv = S.sb("b_pv", [128, NB])
        nx = S.sb("b_nx", [128, NB])
        mup = bcast_row(S, "b_mup", prm['mu_prev'], NB)
        mun = bcast_row(S, "b_mun", prm['mu_next'], NB, q='act')
        w0 = [bcast_row(S, f"b_w0{d}", prm['w0'][d], 1024) for d in range(2)]
        a0 = [bcast_row(S, f"b_a0{d}", prm['a0'][d], 1024, q='act') for d in range(2)]
        kkb = bcast_row(S, "b_kkb", prm['k_k'], 1024)
        kab = bcast_row(S, "b_kab", prm['k_a'], 1024)
        rkb = bcast_row(S, "b_rkb", prm['r_k'], 1024)
        omka = S.sb("b_omka", [128, 1024])
        S.op('dve', lambda e: e.tensor_scalar(out=omka[:], in0=kab[:], scalar1=-1.0, scalar2=1.0, op0=ALU.mult, op1=ALU.add), reads=[kab], writes=[omka])
        w2t = S.sb("b_w2t", [128, 1024])
        a2t = S.sb("b_a2t", [128, 1024])
        g2t = S.sb("b_g2t", [128, 1024])
        S.dma('sp', w2t[:], prm['w2'].rearrange("d r c -> (d r) c"), reads=[], writes=[w2t])
        S.dma('sp', a2t[:], prm['a2'].rearrange("d r c -> (d r) c"), reads=[], writes=[a2t])
        S.dma('sp', g2t[:], prm['g2'], reads=[], writes=[g2t])
        L = S.sb("b_L", [128, 384])
        LT = S.sb("b_LT", [128, 3, 128])
        wd = [S.sb(f"b_wd{d}", [128, 1024]) for d in range(2)]
        ad = [S.sb(f"b_ad{d}", [128, 1024]) for d in range(2)]
        kd = [S.sb(f"b_kd{d}", [128, 1024]) for d in range(2)]
        bd = [S.sb(f"b_bd{d}", [128, 1024]) for d in range(2)]
        kk = S.sb("b_kk", [128, 1024])
        gg = S.sb("b_gg", [128, 1024])
        tmp = S.sb("b_tmp", [128, 1024])
        ft = [S.sb(f"b_ft{i}", [128, 1024]) for i in range(2)]
        vt = [S.sb(f"b_vt{i}", [128, 2, 8, 64]) for i in range(2)]
        ss = S.sb("b_ss", [128, 16])
        pc = 0
        fc = 0
        pbv = proj_d.ap()[:, OFF_B:OFF_B + NB]
        for i in range(NT):
            r0 = i * 128
            S.dma('sp', y[:], pbv[r0:r0 + 128, :], reads=[proj_d], writes=[y])
            if i == 0:
                S.op('dve', lambda e: e.memset(pv[:], 0.0), writes=[pv])
                S.dma('act', pv[1:128, :], pbv[0:127, :], reads=[proj_d], writes=[pv])
            else:
                S.dma('act', pv[:], pbv[r0 - 1:r0 + 127, :], reads=[proj_d], writes=[pv])
            if i == NT - 1:
                S.op('dve', lambda e: e.memset(nx[:], 0.0), writes=[nx])
                S.dma('sp', nx[0:127, :], pbv[r0 + 1:r0 + 128, :], reads=[proj_d], writes=[nx])
            else:
                S.dma('sp', nx[:], pbv[r0 + 1:r0 + 129, :], reads=[proj_d], writes=[nx])
            tt = lambda o, a, b, op: S.op('dve', lambda e: e.tensor_tensor(out=o[:], in0=a[:], in1=b[:], op=op), reads=[a, b], writes=[o])
            tt(pv, pv, y, ALU.subtract)
            tt(pv, pv, mup, ALU.mult)
            tt(nx, nx, y, ALU.subtract)
            tt(nx, nx, mun, ALU.mult)
            tt(y, y, pv, ALU.add)
            tt(y, y, nx, ALU.add)
            r_ = y[:, 0:1024]
            k_ = y[:, 1024:2048]
            v_ = y[:, 2048:3072]
            S.op('act', lambda e: e.activation(out=L[:, 0:128], in_=y[:, 3072:3200], func=AF.Tanh), reads=[y], writes=[L])
            S.op('act', lambda e: e.copy(L[:, 128:256], y[:, 3200:3328]), reads=[y], writes=[L])
            S.op('act', lambda e: e.activation(out=L[:, 256:384], in_=y[:, 3328:3456], func=AF.Sigmoid), reads=[y], writes=[L])
            p = PS[6]
            for j in range(3):
                S.op('pe', lambda e: e.transpose(p[:, j * 128:(j + 1) * 128], L[:, j * 128:(j + 1) * 128], ident[:]), reads=[L, ident], writes=[p])
            S.op('dve', lambda e: e.tensor_copy(LT[:].rearrange("p j t -> p (j t)"), p[:, 0:384]), reads=[p], writes=[LT])
            for d in range(2):
                for half in range(2):
                    cs = slice(half * 512, (half + 1) * 512)
                    a = PS[pc % 4]
                    pc += 1
                    S.op('pe', lambda e: e.matmul(a[:], LT[d * 64:(d + 1) * 64, 0, :], w2t[d * 64:(d + 1) * 64, cs], start=True, stop=True), reads=[LT, w2t], writes=[a])
                    S.op('dve', lambda e: e.tensor_tensor(out=wd[d][:, cs], in0=a[:], in1=w0[d][:, cs], op=ALU.add), reads=[a, w0[d]], writes=[wd[d]])
                    a = PS[pc % 4]
                    pc += 1
                    S.op('pe', lambda e: e.matmul(a[:], LT[d * 64:(d + 1) * 64, 1, :], a2t[d * 64:(d + 1) * 64, cs], start=True, stop=True), reads=[LT, a2t], writes=[a])
                    S.op('dve', lambda e: e.tensor_tensor(out=ad[d][:, cs], in0=a[:], in1=a0[d][:, cs], op=ALU.add), reads=[a, a0[d]], writes=[ad[d]])
                S.op('act', lambda e: e.activation(out=wd[d][:], in_=wd[d][:], func=AF.Sigmoid), reads=[wd[d]], writes=[wd[d]])
                S.op('act', lambda e: e.activation(out=wd[d][:], in_=wd[d][:], func=AF.Exp, scale=-float(np.exp(-0.5))), reads=[wd[d]], writes=[wd[d]])
                S.op('act', lambda e: e.activation(out=ad[d][:], in_=ad[d][:], func=AF.Sigmoid), reads=[ad[d]], writes=[ad[d]])
            for half in range(2):
                cs = slice(half * 512, (half + 1) * 512)
                a = PS[pc % 4]
                pc += 1
                S.op('pe', lambda e: e.matmul(a[:], LT[:, 2, :], g2t[:, cs], start=True, stop=True), reads=[LT, g2t], writes=[a])
                S.op('act', lambda e: e.copy(gg[:, cs], a[:]), reads=[a], writes=[gg])
            S.dma('act', gg_d.ap()[r0:r0 + 128, :], gg[:], reads=[gg], writes=[gg_d])
            S.op('dve', lambda e: e.tensor_tensor(out=kk[:], in0=k_, in1=kkb[:], op=ALU.mult), reads=[y, kkb], writes=[kk])
            S.op('dve', lambda e: e.tensor_tensor(out=tmp[:], in0=kk[:], in1=kk[:], op=ALU.mult), reads=[kk], writes=[tmp])
            S.op('dve', lambda e: e.tensor_reduce(out=ss[:], in_=tmp[:].rearrange("p (h k) -> p h k", k=64), axis=AX.X, op=ALU.add), reads=[tmp], writes=[ss])
            S.op('dve', lambda e: e.tensor_scalar(out=ss[:], in0=ss[:], scalar1=1e-12, scalar2=None, op0=ALU.add), reads=[ss], writes=[ss])
            S.op('act', lambda e: e.activation(out=ss[:], in_=ss[:], func=AF.Sqrt), reads=[ss], writes=[ss])
            S.op('dve', lambda e: e.reciprocal(out=ss[:], in_=ss[:]), reads=[ss], writes=[ss])
            S.op('dve', lambda e: e.tensor_tensor(out=kk[:].rearrange("p (h k) -> p h k", k=64), in0=kk[:].rearrange("p (h k) -> p h k", k=64), in1=ss[:].unsqueeze(2).broadcast_to([128, 16, 64]), op=ALU.mult), reads=[kk, ss], writes=[kk])
            for d in range(2):
                tt(kd[d], ad[d], kab, ALU.mult)
                tt(kd[d], kd[d], omka, ALU.add)
                S.op('dve', lambda e: e.tensor_tensor(out=kd[d][:], in0=kd[d][:], in1=k_, op=ALU.mult), reads=[kd[d], y], writes=[kd[d]])
                tt(bd[d], kk, ad[d], ALU.mult)
                S.op('dve', lambda e: e.tensor_scalar(out=bd[d][:], in0=bd[d][:], scalar1=-1.0, scalar2=None, op0=ALU.mult), reads=[bd[d]], writes=[bd[d]])
            tt(tmp, kd[0], kd[1], ALU.add)
            S.op('dve', lambda e: e.tensor_tensor(out=tmp[:], in0=tmp[:], in1=r_, op=ALU.mult), reads=[tmp, y], writes=[tmp])
            tt(tmp, tmp, rkb, ALU.mult)
            S.op('dve', lambda e: e.tensor_reduce(out=ss[:], in_=tmp[:].rearrange("p (h k) -> p h k", k=64), axis=AX.X, op=ALU.add), reads=[tmp], writes=[ss])
            S.op('dve', lambda e: e.tensor_tensor(out=tmp[:].rearrange("p (h k) -> p h k", k=64), in0=y[:, 2048:3072].rearrange("p (h k) -> p h k", k=64), in1=ss[:].unsqueeze(2).broadcast_to([128, 16, 64]), op=ALU.mult), reads=[y, ss], writes=[tmp])
            S.dma('act', bon_d.ap()[r0:r0 + 128, :], tmp[:], reads=[tmp], writes=[bon_d])
            for d in range(2):
                srcs = [(kk, kk[:]), (wd[d], wd[d][:]), (bd[d], bd[d][:]), (kd[d], kd[d][:]), (y, r_)]
                for X, (sb_, sap) in enumerate(srcs):
                    f_ = ft[fc % 2]
                    fc += 1
                    fv = f_[:].rearrange("p (h q k) -> p h q k", h=2, q=8)
                    if d == 0:
                        rr = r0
                        S.op('act', lambda e: e.copy(fv.rearrange("p h q k -> p q h k"), sap.rearrange("p (q h k) -> p q h k", q=8, h=2)), reads=[sb_], writes=[f_])
                    else:
                        rr = (NT - 1 - i) * 128
                        for half in range(2):
                            cs = slice(half * 512, (half + 1) * 512)
                            a = PS[pc % 4]
                            pc += 1
                            S.op('pe', lambda e: e.matmul(a[:], J[:], sap[:, cs], start=True, stop=True), reads=[J, sb_], writes=[a])
                            S.op('act', lambda e: e.copy(fv[:, :, half * 4:(half + 1) * 4, :].rearrange("p h q k -> p q h k"), a[:].rearrange("p (q h k) -> p q h k", q=4, h=2)), reads=[a], writes=[f_])
                    for h2 in range(2):
                        S.dma('sp', Xd.ap()[d, rr:rr + 128, h2, X, :, :], fv[:, h2, :, :], reads=[f_], writes=[Xd])
            for d in range(2):
                v2 = vt[d]
                for qg in range(2):
                    p = PS[4 + qg]
                    for j in range(4):
                        hq = qg * 4 + j
                        if d == 0:
                            S.op('pe', lambda e: e.transpose(p[:, j * 128:(j + 1) * 128], y[:, 2048 + hq * 128:2048 + (hq + 1) * 128], ident[:]), reads=[y, ident], writes=[p])
                        else:
                            S.op('pe', lambda e: e.matmul(p[:, j * 128:(j + 1) * 128], y[:, 2048 + hq * 128:2048 + (hq + 1) * 128], J[:], start=True, stop=True), reads=[y, J], writes=[p])
                    S.op('dve', lambda e: e.tensor_copy(v2[:, :, qg * 4:qg * 4 + 4, :].rearrange("p c j t -> p j c t"), p[:].rearrange("p (j c t) -> p j c t", j=4, c=2)), reads=[p], writes=[v2])
                ti = i if d == 0 else (NT - 1 - i)
                S.dma('sp', VTd.ap()[d, :, 2 * ti:2 * ti + 2, :, :], v2[:], reads=[v2], writes=[VTd])
    if parts[1]:
      with S.scope():
        Sel = S.sb("s_sel", [128, 64, 128])
        S.dma('sp', Sel[:], sel_d.ap(), reads=[sel_d], writes=[Sel])
        R = [S.sb(f"s_R{i}", [128, 2, 5, 512]) for i in range(2)]
        vtc = [S.sb(f"s_vt{i}", [128, 16, 64]) for i in range(2)]
        Ob = [S.sb(f"s_Ob{i}", [128, 16, 64]) for i in range(2)]
        St = [S.sb(f"s_St{d}", [128, 512]) for d in range(2)]
        t1 = S.sb("s_t1", [128, 512])
        t2 = S.sb("s_t2", [128, 512])
        sa = S.sb("s_sa", [128, 8])
        for d in range(2):
            S.op('dve', lambda e: e.memset(St[d][:], 0.0), writes=[St[d]])
        bkc = 0
        v3 = lambda ap: ap.rearrange("p (q k) -> p q k", k=64)
        for c in range(SEQ // 64):
            Rc = R[c % 2]
            vc_ = vtc[c % 2]
            ob = Ob[c % 2]
            for d in range(2):
                for h2 in range(2):
                    S.dma('sp' if d == 0 else 'act', Rc[h2 * 64:(h2 + 1) * 64, d, :, :], Xd.ap()[d, c * 64:(c + 1) * 64, h2, :, :, :].rearrange("t x q k -> t x (q k)"), reads=[Xd], writes=[Rc])
                S.dma('sp', vc_[:, d * 8:(d + 1) * 8, :], VTd.ap()[d, :, c, :, :], reads=[VTd], writes=[vc_])
            for tl_ in range(64):
                for d in range(2):
                    bk = []
                    for X in range(5):
                        b = PS[bkc % 8]
                        bkc += 1
                        S.op('pe', lambda e: e.matmul(b[:], Sel[:, tl_, :], Rc[:, d, X, :], start=True, stop=True), reads=[Sel, Rc], writes=[b])
                        bk.append(b)
                    Sd = St[d]
                    S.op('dve', lambda e: e.tensor_tensor(out=t1[:], in0=Sd[:], in1=bk[0][:], op=ALU.mult), reads=[Sd, bk[0]], writes=[t1])
                    S.op('dve', lambda e: e.tensor_reduce(out=sa[:], in_=v3(t1[:]), axis=AX.X, op=ALU.add), reads=[t1], writes=[sa])
                    S.op('dve', lambda e: e.tensor_tensor(out=Sd[:], in0=Sd[:], in1=bk[1][:], op=ALU.mult), reads=[Sd, bk[1]], writes=[Sd])
                    S.op('dve', lambda e: e.tensor_tensor(out=v3(t2[:]), in0=v3(bk[2][:]), in1=sa[:].unsqueeze(2).broadcast_to([128, 8, 64]), op=ALU.mult), reads=[bk[2], sa], writes=[t2])
                    S.op('dve', lambda e: e.tensor_tensor(out=Sd[:], in0=Sd[:], in1=t2[:], op=ALU.add), reads=[Sd, t2], writes=[Sd])
                    S.op('dve', lambda e: e.tensor_tensor(out=v3(t2[:]), in0=v3(bk[3][:]), in1=vc_[:, d * 8:(d + 1) * 8, tl_:tl_ + 1].broadcast_to([128, 8, 64]), op=ALU.mult), reads=[bk[3], vc_], writes=[t2])
                    S.op('dve', lambda e: e.tensor_tensor(out=Sd[:], in0=Sd[:], in1=t2[:], op=ALU.add), reads=[Sd, t2], writes=[Sd])
                    S.op('dve', lambda e: e.tensor_tensor(out=t1[:], in0=Sd[:], in1=bk[4][:], op=ALU.mult), reads=[Sd, bk[4]], writes=[t1])
                    col = tl_ if d == 0 else 63 - tl_
                    S.op('dve', lambda e: e.tensor_reduce(out=ob[:, d * 8:(d + 1) * 8, col:col + 1], in_=v3(t1[:]), axis=AX.X, op=ALU.add), reads=[t1], writes=[ob])
            S.dma('sp', Od.ap()[0, :, c, :, :], ob[:, 0:8, :], reads=[ob], writes=[Od])
            S.dma('sp', Od.ap()[1, :, SEQ // 64 - 1 - c, :, :], ob[:, 8:16, :], reads=[ob], writes=[Od])
    if parts[2]:
      with S.scope():
        gng = bcast_row(S, "f_gng", prm['gn_g'], 1024)
        gnb = bcast_row(S, "f_gnb", prm['gn_b'], 1024)
        Ot = [S.sb(f"f_Ot{i}", [128, 2, 2, 8, 64]) for i in range(2)]
        osum = S.sb("f_osum", [128, 8, 128])
        o = S.sb("f_o", [128, 1024])
        xc = S.sb("f_xc", [128, 1024])
        sq = S.sb("f_sq", [128, 1024])
        bt_ = [S.sb(f"f_bon{i}", [128, 1024]) for i in range(2)]
        gt_ = [S.sb(f"f_g{i}", [128, 1024]) for i in range(2)]
        st = S.sb("f_st", [128, 2, 16])
        h3 = lambda ap: ap.rearrange("p (h k) -> p h k", k=64)
        for i in range(NT):
            r0 = i * 128
            ot = Ot[i % 2]
            bo = bt_[i % 2]
            gg = gt_[i % 2]
            for d in range(2):
                S.dma('sp', ot[:, d], Od.ap()[d, :, 2 * i:2 * i + 2, :, :], reads=[Od], writes=[ot])
            S.dma('act', bo[:], bon_d.ap()[r0:r0 + 128, :], reads=[bon_d], writes=[bo])
            S.dma('act', gg[:], gg_d.ap()[r0:r0 + 128, :], reads=[gg_d], writes=[gg])
            for cc in range(2):
                S.op('dve', lambda e: e.tensor_tensor(out=osum[:, :, cc * 64:(cc + 1) * 64], in0=ot[:, 0, cc], in1=ot[:, 1, cc], op=ALU.add), reads=[ot], writes=[osum])
            for qg in range(2):
                p = PS[qg]
                for j in range(4):
                    hq = qg * 4 + j
                    S.op('pe', lambda e: e.transpose(p[:, j * 128:(j + 1) * 128], osum[:, hq, :], ident[:]), reads=[osum, ident], writes=[p])
                S.op('dve', lambda e: e.tensor_copy(o[:, qg * 512:(qg + 1) * 512], p[:]), reads=[p], writes=[o])
            S.op('dve', lambda e: e.tensor_reduce(out=st[:, 0, :], in_=h3(o[:]), axis=AX.X, op=ALU.add), reads=[o], writes=[st])
            S.op('dve', lambda e: e.tensor_scalar(out=st[:, 0, :], in0=st[:, 0, :], scalar1=-1.0 / 64, scalar2=None, op0=ALU.mult), reads=[st], writes=[st])
            S.op('dve', lambda e: e.tensor_tensor(out=h3(xc[:]), in0=h3(o[:]), in1=st[:, 0, :].unsqueeze(2).broadcast_to([128, 16, 64]), op=ALU.add), reads=[o, st], writes=[xc])
            S.op('dve', lambda e: e.tensor_tensor(out=sq[:], in0=xc[:], in1=xc[:], op=ALU.mult), reads=[xc], writes=[sq])
            S.op('dve', lambda e: e.tensor_reduce(out=st[:, 1, :], in_=h3(sq[:]), axis=AX.X, op=ALU.add), reads=[sq], writes=[st])
            S.op('dve', lambda e: e.tensor_scalar(out=st[:, 1, :], in0=st[:, 1, :], scalar1=1.0 / 64, scalar2=64e-5, op0=ALU.mult, op1=ALU.add), reads=[st], writes=[st])
            S.op('act', lambda e: e.activation(out=st[:, 1, :], in_=st[:, 1, :], func=AF.Sqrt), reads=[st], writes=[st])
            S.op('dve', lambda e: e.reciprocal(out=st[:, 1, :], in_=st[:, 1, :]), reads=[st], writes=[st])
            S.op('dve', lambda e: e.tensor_tensor(out=h3(xc[:]), in0=h3(xc[:]), in1=st[:, 1, :].unsqueeze(2).broadcast_to([128, 16, 64]), op=ALU.mult), reads=[xc, st], writes=[xc])
            S.op('dve', lambda e: e.tensor_tensor(out=xc[:], in0=xc[:], in1=gng[:], op=ALU.mult), reads=[xc, gng], writes=[xc])
            S.op('dve', lambda e: e.tensor_tensor(out=xc[:], in0=xc[:], in1=gnb[:], op=ALU.add), reads=[xc, gnb], writes=[xc])
            S.op('dve', lambda e: e.tensor_tensor(out=xc[:], in0=xc[:], in1=bo[:], op=ALU.add), reads=[xc, bo], writes=[xc])
            S.op('dve', lambda e: e.tensor_tensor(out=bo[:], in0=xc[:], in1=gg[:], op=ALU.mult), reads=[xc, gg], writes=[bo])
            S.dma('sp', yb_d.ap()[r0:r0 + 128, :], bo[:], reads=[bo], writes=[yb_d])


VEC_NAMES = ['mu_prev', 'mu_next', 'rwkv_w0', 'rwkv_w2', 'rwkv_a0', 'rwkv_a2', 'rwkv_g2', 'rwkv_k_k', 'rwkv_k_a', 'rwkv_r_k',
             'rwkv_gn_g', 'rwkv_gn_b', 'q_norm', 'k_norm', 'ln1_g', 'ln1_b', 'ln2_g', 'ln2_b']


def build_full(SEQ, L, NGRP, shapes, dbg=False):
    global ALPHA
    ALPHA = (2.0 * L) ** 0.25
    NE = NGRP * 8
    NR = NGRP + NE
    nc = bass.Bass("TRN2", target_bir_lowering=False)
    with ExitStack() as es:
        S = Sched(nc, es)
        I = lambda n, sh: S.dram(n, list(sh), F32, kind="ExternalInput")
        x_in = I("x", [SEQ, D])
        w_in = I("w_in", [L, D, IN_COLS])
        w_branch = I("w_branch", [L, 2560, D])
        w_out = I("w_out", [L, D, D])
        vec = {n: I(n, shapes[n]) for n in VEC_NAMES}
        wr = I("wr", [L, D, NR])
        rb = I("rb", [L, NR])
        wg = I("w_gate", [L, NE, D, FF])
        wu = I("w_up", [L, NE, D, FF])
        wd = I("w_down", [L, NE, FF, D])
        bt = I("bt", [12, 2, 128, 256])
        cos = I("cos", [SEQ, 128])
        sin = I("sin", [SEQ, 128])
        sel = I("sel", [128, 64, 128])
        identd = I("identd", [128, 128])
        Jd = I("Jd", [128, 128])
        out = S.dram("out", [SEQ, D], F32, kind="ExternalOutput")
        DBG = ("proj", "ya", "yb", "yc", "x1") if dbg else ()
        T = lambda n, sh: S.dram(n, list(sh), F32, kind="ExternalOutput" if n in DBG else "Internal")
        proj = T("proj", [SEQ, IN_COLS])
        qkn = T("qkn", [SEQ, 1280])
        ya = T("ya", [SEQ, 1024])
        yb = T("yb", [SEQ, 1024])
        yc = T("yc", [SEQ, 512])
        nz = T("nz", [3, SEQ, 4, 129])
        Xd = T("Xd", [2, SEQ, 2, 5, 8, 64])
        VTd = T("VTd", [2, 128, SEQ // 64, 8, 64])
        Od = T("Od", [2, 128, SEQ // 64, 8, 64])
        bon = T("bon", [SEQ, 1024])
        ggd = T("ggd", [SEQ, 1024])
        x1 = T("x1", [SEQ, D])
        x1T = T("x1T", [16, 128, SEQ])
        Gd = T("Gd", [SEQ, NE])
        xs = [T("xA", [SEQ, D]), T("xB", [SEQ, D])]
        ident = S.sb("ident", [128, 128])
        J = S.sb("J", [128, 128])
        S.dma('sp', ident[:], identd.ap(), reads=[identd], writes=[ident])
        S.dma('sp', J[:], Jd.ap(), reads=[Jd], writes=[J])
        PS = [S.ps(f"ps{i}", [128, 512]) for i in range(8)]
        xc = x_in
        for l in range(L):
            xo = out if l == L - 1 else xs[l % 2]
            _w = _APBuf(w_in, l)
            phase_proj(S, xc, _w, proj, ident, PS, SEQ)
            phase_attn_a(S, proj, ya, qkn, cos, sin, vec['q_norm'].ap()[l], vec['k_norm'].ap()[l], ident, PS, SEQ)
            prm = dict(mu_prev=vec['mu_prev'].ap()[l], mu_next=vec['mu_next'].ap()[l], w0=vec['rwkv_w0'].ap()[l], w2=vec['rwkv_w2'].ap()[l],
                       a0=vec['rwkv_a0'].ap()[l], a2=vec['rwkv_a2'].ap()[l], g2=vec['rwkv_g2'].ap()[l], k_k=vec['rwkv_k_k'].ap()[l],
                       k_a=vec['rwkv_k_a'].ap()[l], r_k=vec['rwkv_r_k'].ap()[l], gn_g=vec['rwkv_gn_g'].ap()[l], gn_b=vec['rwkv_gn_b'].ap()[l])
            phase_rwkv(S, proj, yb, Xd, VTd, Od, bon, ggd, prm, sel, ident, J, PS, SEQ)
            phase_dilated(S, proj, nz, yc, bt, ident, PS, SEQ)
            phase_merge(S, ya, yb, yc, proj, xc, w_branch.ap()[l], w_out.ap()[l], vec['ln1_g'].ap()[l], vec['ln1_b'].ap()[l],
                        wr.ap()[l], rb.ap()[l], x1, x1T, Gd, ident, PS, SEQ, NGRP)
            phase_moe(S, x1, x1T, Gd, wg.ap()[l], wu.ap()[l], wd.ap()[l], vec['ln2_g'].ap()[l], vec['ln2_b'].ap()[l], xo, PS, SEQ, NGRP)
            xc = xo
        S.finish([out], 'sp')
        S.barrier()
        print("ninst", S.ninst, flush=True)
    return nc


class _APBuf:
    def __init__(self, buf, l):
        self.buf = buf
        self.l = l
        self.dram = True
        self.w = buf.w
        self.r = buf.r
        self.name = buf.name

    def ap(self):
        return self.buf.ap()[self.l]


def host_inputs(inputs, b, SEQ, L, NGRP):
    f = lambda a: np.ascontiguousarray(a, dtype=np.float32)
    m = {"x": f(inputs["x"][b, :SEQ])}
    for n in ["w_in", "w_branch", "w_out", "w_gate", "w_up", "w_down"] + VEC_NAMES:
        m[n] = f(inputs[n][:L])
    NE = NGRP * 8
    m["w_gate"] = f(inputs["w_gate"][:L, :NE])
    m["w_up"] = f(inputs["w_up"][:L, :NE])
    m["w_down"] = f(inputs["w_down"][:L, :NE])
    m["wr"] = f(np.concatenate([inputs["router_group_w"][:L, :, :NGRP], inputs["router_expert_w"][:L, :, :NE]], axis=2))
    m["rb"] = f(np.concatenate([inputs["router_group_b"][:L, :NGRP], inputs["router_expert_b"][:L, :NE]], axis=1))
    m["bt"] = bias_tables(np.asarray(inputs["rel_bias"], dtype=np.float32))
    COS, SINS = rope_tables(SEQ)
    m["cos"] = COS
    m["sin"] = SINS
    m["sel"] = sel_table()
    m["identd"] = np.eye(128, dtype=np.float32)
    m["Jd"] = anti_ident()
    return m


from concourse.bass_utils import run_bass_kernel_spmd

_SEQ, _L, _NGRP, _NB = 4096, 4, 8, 4


def kernel(**inputs):
    inputs = {k: np.asarray(v) for k, v in inputs.items()}
    shapes = {n: tuple(inputs[n].shape) for n in VEC_NAMES}
    nc = build_full(_SEQ, _L, _NGRP, shapes)
    ims = [host_inputs(inputs, b, _SEQ, _L, _NGRP) for b in range(_NB)]
    res = run_bass_kernel_spmd(nc, ims, core_ids=list(range(_NB)))
    return np.stack([np.asarray(res.results[b]["out"], dtype=np.float32) for b in range(_NB)], axis=0)
```

```python
import numpy as np
from contextlib import ExitStack
import concourse.bass as bass
import concourse.mybir as mybir

F32 = mybir.dt.float32
BF16 = mybir.dt.bfloat16
I32 = mybir.dt.int32
U32 = mybir.dt.uint32
AF = mybir.ActivationFunctionType
ALU = mybir.AluOpType
AX = mybir.AxisListType

SAME_ENGINE_SYNC = True


class Buf:
    def __init__(self, S, t, name, dram=False):
        self.S = S
        self.t = t
        self.name = name
        self.dram = dram
        self.w = {}
        self.r = {}
        self.dsem = None
        self.dcount = 0

    def __getitem__(self, idx):
        return self.t[idx]

    def ap(self):
        return self.t.ap() if self.dram else self.t[:]


class Sched:
    def __init__(self, nc, es):
        self.nc = nc
        self.es = es
        self.eng = {'pe': nc.tensor, 'dve': nc.vector, 'act': nc.scalar, 'pool': nc.gpsimd, 'sp': nc.sync}
        self.sem = {}
        self.cnt = {}
        self.known = {}
        for e in self.eng:
            self.sem[e] = es.enter_context(nc.semaphore('s_' + e))
            self.cnt[e] = 0
            self.known[e] = {}
        self.dma_owner = {}
        self.nbuf = 0
        self.ninst = 0
        self.stacks = [es]
        self.free_dsems = []

    def scope(self):
        S = self

        class _Scope:
            def __enter__(s2):
                s2.st = ExitStack()
                s2.st.__enter__()
                S.stacks.append(s2.st)
                s2.owners0 = set(S.dma_owner.keys())
                return s2

            def __exit__(s2, *a):
                S.barrier()
                for k in list(S.dma_owner.keys()):
                    if k not in s2.owners0:
                        b = S.dma_owner.pop(k)
                        S.free_dsems.append((b.dsem, b.dcount))
                S.stacks.pop()
                return s2.st.__exit__(*a)
        return _Scope()

    def barrier(self):
        toks = {}
        for e in self.eng:
            if self.cnt[e] > 0:
                toks[id(self.sem[e])] = (self.sem[e], self.cnt[e])
        for sid, b in self.dma_owner.items():
            if b.dcount > 0:
                toks[sid] = (b.dsem, b.dcount)
        for e in self.eng:
            self._need(e, toks)

    def sb(self, name, shape, dt=F32):
        self.nbuf += 1
        name = "%s_%d" % (name, self.nbuf)
        t = self.stacks[-1].enter_context(self.nc.sbuf_tensor(name, list(shape), dt))
        return Buf(self, t, name)

    def ps(self, name, shape, dt=F32):
        t = self.es.enter_context(self.nc.psum_tensor(name, list(shape), dt))
        return Buf(self, t, name)

    def dram(self, name, shape, dt=F32, kind='Internal'):
        t = self.nc.dram_tensor(name, list(shape), dt, kind=kind)
        return Buf(self, t, name, dram=True)

    def _need(self, e, toks):
        own = id(self.sem[e]) if e in self.sem else None
        for sid, (sem, val) in toks.items():
            if sid == own and (not SAME_ENGINE_SYNC or e == 'pe'):
                continue
            k = self.known[e].get(sid, 0)
            if k >= val:
                continue
            owner = self.dma_owner.get(sid)
            if owner is not None:
                val = owner.dcount
            self.eng[e].wait_ge(sem, val)
            self.known[e][sid] = val

    @staticmethod
    def _merge(dst, toks):
        for sid, (sem, val) in toks.items():
            if sid not in dst or dst[sid][1] < val:
                dst[sid] = (sem, val)

    def _deps(self, e, reads, writes):
        toks = {}
        for b in reads:
            self._merge(toks, b.w)
        for b in writes:
            self._merge(toks, b.w)
            self._merge(toks, b.r)
        self._need(e, toks)

    def _commit(self, tok, reads, writes):
        sid = id(tok[0])
        for b in reads:
            if sid not in b.r or b.r[sid][1] < tok[1]:
                b.r[sid] = tok
        for b in writes:
            b.r = {}
            if sid not in b.w or b.w[sid][1] < tok[1]:
                b.w[sid] = tok

    def op(self, e, fn, reads=(), writes=()):
        self._deps(e, reads, writes)
        inst = fn(self.eng[e])
        self.cnt[e] += 1
        inst.then_inc(self.sem[e], 1)
        self.ninst += 1
        self._commit((self.sem[e], self.cnt[e]), reads, writes)
        return inst

    def dma(self, q, out_ap, in_ap, reads=(), writes=(), sbuf=None, **kw):
        if sbuf is None:
            for b in list(writes) + list(reads):
                if not b.dram:
                    sbuf = b
                    break
        if sbuf is None:
            sbuf = writes[0]
        if sbuf.dsem is None:
            if self.free_dsems:
                sbuf.dsem, sbuf.dcount = self.free_dsems.pop()
            else:
                sbuf.dsem = self.es.enter_context(self.nc.semaphore('d%d' % len(self.dma_owner) + sbuf.name))
            self.dma_owner[id(sbuf.dsem)] = sbuf
        self._deps(q, reads, writes)
        inst = self.eng[q].dma_start(out=out_ap, in_=in_ap, **kw)
        sbuf.dcount += 16
        inst.then_inc(sbuf.dsem, 16)
        self.ninst += 1
        self._commit((sbuf.dsem, sbuf.dcount), reads, writes)
        return inst

    def finish(self, bufs, e='sp'):
        toks = {}
        for b in bufs:
            self._merge(toks, b.w)
        self._need(e, toks)


def rope_tables(SEQ):
    pos = np.arange(SEQ)
    row = (pos // 64).astype(np.float32); col = (pos % 64).astype(np.float32)
    inv = (10000.0 ** (-np.arange(0, 64, 2, dtype=np.float32) / 64)).astype(np.float32)
    ar = row[:, None] * inv[None, :]; ac = col[:, None] * inv[None, :]
    cr, sr, cc, sc = np.cos(ar), np.sin(ar), np.cos(ac), np.sin(ac)
    COS = np.concatenate([cr, cr, cc, cc], 1).astype(np.float32)
    SINS = np.concatenate([-sr, sr, -sc, sc], 1).astype(np.float32)
    return COS, SINS
def t5_bucket(rel):
    nb = 16; max_exact = 8
    n = np.abs(rel)
    large = max_exact + (np.log(np.maximum(n, 1) / max_exact) / np.log(1024 / max_exact) * (nb - max_exact)).astype(np.int32)
    large = np.minimum(large, nb - 1)
    return (rel > 0).astype(np.int32) * nb + np.where(n < max_exact, n, large)
def bias_tables(rel_bias):
    out = np.full((12, 2, 128, 256), -30000.0, np.float32)
    p = np.arange(128)[:, None]; f = np.arange(256)[None, :]
    delta = 64 + p - f
    mask = np.abs(delta) <= 64
    for g, d in enumerate((1, 4, 16)):
        b = t5_bucket(delta * d)
        for h in range(4):
            hh = g * 4 + h
            vals = rel_bias[b, hh]
            out[hh, 0] = np.where(mask, vals, -30000.0)
            out[hh, 1, 0:64] = out[hh, 0, 64:128]
    return out
def sel_table():
    s = np.zeros((128, 64, 128), np.float32)
    for h2 in range(2):
        for t in range(64):
            s[h2 * 64 + t, t, h2 * 64:(h2 + 1) * 64] = 1.0
    return s
def anti_ident():
    return np.ascontiguousarray(np.eye(128, dtype=np.float32)[::-1])


D = 2048
A_Q, A_KV = 1024, 256
A_COLS = 1536
B_COLS = 3456
C_QKV = 1536
C_COLS = 4608
G_COLS = 6144
IN_COLS = 15744
OFF_B = A_COLS
OFF_C = A_COLS + B_COLS
OFF_G = OFF_C + C_COLS
NEXP = 64
FF = 384
ALPHA = 8.0 ** 0.25
C_DIL = (1, 4, 16)


def common(S_, nc):
    pass


def phase_proj(S, x_d, w_d, proj_d, ident, PS, SEQ):
    KC = D // 128
    TG = 1024 if SEQ % 1024 == 0 else 512
    with S.scope():
        xT = S.sb("p1_xT", [128, KC, TG])
        xin = [S.sb(f"p1_xin{i}", [128, D]) for i in range(2)]
        wt = [S.sb(f"p1_wt{i}", [128, KC, 512]) for i in range(2)]
        ot = [S.sb(f"p1_ot{i}", [128, 512]) for i in range(4)]
        wv = w_d.ap().rearrange("(k p) c -> p k c", p=128)
        nblk = (IN_COLS + 511) // 512
        cnt = 0
        wcnt = 0
        for tg in range(SEQ // TG):
            for m in range(TG // 128):
                xi = xin[m % 2]
                r0 = tg * TG + m * 128
                S.dma('sp', xi[:], x_d.ap()[r0:r0 + 128, :], reads=[x_d], writes=[xi])
                for kg in range(KC // 4):
                    p = PS[6 + kg % 2]
                    for j in range(4):
                        k = kg * 4 + j
                        S.op('pe', lambda e: e.transpose(p[:, j * 128:(j + 1) * 128], xi[:, k * 128:(k + 1) * 128], ident[:]), reads=[xi, ident], writes=[p])
                    S.op('dve', lambda e: e.tensor_copy(xT[:, kg * 4:kg * 4 + 4, m * 128:(m + 1) * 128], p[:].rearrange("p (j t) -> p j t", j=4)), reads=[p], writes=[xT])
            for n in range(nblk):
                c0 = n * 512
                cw = min(512, IN_COLS - c0)
                wb = wt[wcnt % 2]
                wcnt += 1
                S.dma('act' if n % 2 else 'sp', wb[:, :, 0:cw], wv[:, :, c0:c0 + cw], reads=[w_d], writes=[wb])
                for m in range(TG // 128):
                    a = PS[cnt % 4]
                    o = ot[cnt % 4]
                    cnt += 1
                    for k in range(KC):
                        S.op('pe', lambda e: e.matmul(a[:, 0:cw], xT[:, k, m * 128:(m + 1) * 128], wb[:, k, 0:cw], start=(k == 0), stop=(k == KC - 1)), reads=[xT, wb], writes=[a])
                    if cnt % 2:
                        S.op('act', lambda e: e.copy(o[:, 0:cw], a[:, 0:cw]), reads=[a], writes=[o])
                    else:
                        S.op('dve', lambda e: e.tensor_copy(o[:, 0:cw], a[:, 0:cw]), reads=[a], writes=[o])
                    r0 = tg * TG + m * 128
                    S.dma('sp', proj_d.ap()[r0:r0 + 128, c0:c0 + cw], o[:, 0:cw], reads=[o], writes=[proj_d])


def phase_attn_a(S, proj_d, ya_d, qkn_d, cos_d, sin_d, qg_ap, kg_ap, ident, PS, SEQ):
    NT = SEQ // 128
    with S.scope():
        gq = S.sb("a_gq", [128, 128])
        gk = S.sb("a_gk", [128, 128])
        S.dma('sp', gq[:], qg_ap.partition_broadcast(128), reads=[], writes=[gq])
        S.dma('sp', gk[:], kg_ap.partition_broadcast(128), reads=[], writes=[gk])
        tin = [S.sb(f"a_tin{i}", [128, 1280]) for i in range(2)]
        cs = [S.sb(f"a_cs{i}", [128, 2, 128]) for i in range(2)]
        sq = S.sb("a_sq", [128, 1280])
        tmp = S.sb("a_tmp", [128, 1280])
        ss = S.sb("a_ss", [128, 10])
        for i in range(NT):
            t = tin[i % 2]
            c = cs[i % 2]
            r0 = i * 128
            S.dma('sp', t[:], proj_d.ap()[r0:r0 + 128, 0:1280], reads=[proj_d], writes=[t])
            S.dma('act', c[:, 0, :], cos_d.ap()[r0:r0 + 128, :], reads=[cos_d], writes=[c])
            S.dma('act', c[:, 1, :], sin_d.ap()[r0:r0 + 128, :], reads=[sin_d], writes=[c])
            S.op('dve', lambda e: e.tensor_tensor(out=sq[:], in0=t[:], in1=t[:], op=ALU.mult), reads=[t], writes=[sq])
            S.op('dve', lambda e: e.tensor_reduce(out=ss[:], in_=sq[:].rearrange("p (h d) -> p h d", h=10), axis=AX.X, op=ALU.add), reads=[sq], writes=[ss])
            S.op('dve', lambda e: e.tensor_scalar(out=ss[:], in0=ss[:], scalar1=1.0 / 128, scalar2=1e-6, op0=ALU.mult, op1=ALU.add), reads=[ss], writes=[ss])
            S.op('act', lambda e: e.activation(out=ss[:], in_=ss[:], func=AF.Sqrt), reads=[ss], writes=[ss])
            S.op('dve', lambda e: e.reciprocal(out=ss[:], in_=ss[:]), reads=[ss], writes=[ss])
            for h in range(10):
                g = gq if h < 8 else gk
                S.op('dve', lambda e: e.scalar_tensor_tensor(out=t[:, h * 128:(h + 1) * 128], in0=t[:, h * 128:(h + 1) * 128], scalar=ss[:, h:h + 1], in1=g[:], op0=ALU.mult, op1=ALU.mult), reads=[t, ss, g], writes=[t])
            tv = t[:].rearrange("p (h a b f) -> p h a b f", h=10, a=2, b=2)
            mv = tmp[:].rearrange("p (h a b f) -> p h a b f", h=10, a=2, b=2)
            sv = c[:, 1, :].rearrange("p (a b f) -> p a b f", a=2, b=2)
            for b in range(2):
                S.op('dve', lambda e: e.tensor_tensor(out=mv[:, :, :, b, :], in0=tv[:, :, :, 1 - b, :], in1=sv[:, :, b, :].unsqueeze(1).broadcast_to([128, 10, 2, 32]), op=ALU.mult), reads=[t, c], writes=[tmp])
            S.op('dve', lambda e: e.tensor_tensor(out=t[:].rearrange("p (h d) -> p h d", h=10), in0=t[:].rearrange("p (h d) -> p h d", h=10), in1=c[:, 0, :].unsqueeze(1).broadcast_to([128, 10, 128]), op=ALU.mult), reads=[t, c], writes=[t])
            S.op('dve', lambda e: e.tensor_tensor(out=t[:], in0=t[:], in1=tmp[:], op=ALU.add), reads=[t, tmp], writes=[t])
            S.dma('sp', qkn_d.ap()[r0:r0 + 128, :], t[:], reads=[t], writes=[qkn_d])
    with S.scope():
        kT = S.sb("a_kT", [128, SEQ])
        qT = S.sb("a_qT", [128, SEQ])
        V = S.sb("a_V", [128, NT, 129])
        tl = [S.sb(f"a_tl{i}", [128, 128]) for i in range(4)]
        pT = [S.sb(f"a_pT{i}", [128, 512]) for i in range(2)]
        o_sb = [S.sb(f"a_o{i}", [128, 128]) for i in range(2)]
        rec = S.sb("a_rec", [128, 1])
        S.op('dve', lambda e: e.memset(V[:], 1.0), writes=[V])
        tcnt = 0

        def load_T(dst, col0, src_d):
            nonlocal tcnt
            for i in range(NT):
                tt = tl[tcnt % 4]
                p = PS[6 + tcnt % 2]
                tcnt += 1
                S.dma('sp', tt[:], src_d.ap()[i * 128:(i + 1) * 128, col0:col0 + 128], reads=[src_d], writes=[tt])
                S.op('pe', lambda e: e.transpose(p[:, 0:128], tt[:], ident[:]), reads=[tt, ident], writes=[p])
                S.op('dve', lambda e: e.tensor_copy(dst[:, i * 128:(i + 1) * 128], p[:, 0:128]), reads=[p], writes=[dst])

        ocnt = 0
        for kvh in range(2):
            load_T(kT, 1024 + kvh * 128, qkn_d)
            S.dma('sp', V[:, :, 0:128], proj_d.ap()[:, 1280 + kvh * 128:1280 + (kvh + 1) * 128].rearrange("(t p) d -> p t d", p=128), reads=[proj_d], writes=[V])
            for g in range(4):
                h = kvh * 4 + g
                load_T(qT, h * 128, qkn_d)
                for qb in range(SEQ // 512):
                    for j in range(NT):
                        sp_ = PS[4 + j % 2]
                        pt = pT[j % 2]
                        S.op('pe', lambda e: e.matmul(sp_[:], kT[:, j * 128:(j + 1) * 128], qT[:, qb * 512:(qb + 1) * 512], start=True, stop=True), reads=[kT, qT], writes=[sp_])
                        S.op('act', lambda e: e.activation(out=pt[:], in_=sp_[:], func=AF.Exp, scale=128.0 ** -0.5), reads=[sp_], writes=[pt])
                        for sub in range(4):
                            S.op('pe', lambda e: e.matmul(PS[sub][:, 0:129], pt[:, sub * 128:(sub + 1) * 128], V[:, j, :], start=(j == 0), stop=(j == NT - 1)), reads=[pt, V], writes=[PS[sub]])
                    for sub in range(4):
                        o = o_sb[ocnt % 2]
                        ocnt += 1
                        S.op('dve', lambda e: e.reciprocal(out=rec[:], in_=PS[sub][:, 128:129]), reads=[PS[sub]], writes=[rec])
                        S.op('dve', lambda e: e.tensor_scalar(out=o[:], in0=PS[sub][:, 0:128], scalar1=rec[:, 0:1], scalar2=None, op0=ALU.mult), reads=[PS[sub], rec], writes=[o])
                        r0 = qb * 512 + sub * 128
                        S.dma('sp', ya_d.ap()[r0:r0 + 128, h * 128:(h + 1) * 128], o[:], reads=[o], writes=[ya_d])


def bcast_row(S, name, ap_row, n, q='sp'):
    t = S.sb(name, [128, n])
    S.dma(q, t[:], ap_row.partition_broadcast(128), reads=[], writes=[t])
    return t


def layer_norm(S, x, xs, g_bc, b_bc, out, outs, scr, st, eps=1e-5):
    S.op('dve', lambda e: e.tensor_reduce(out=st[:, 0:1], in_=xs, axis=AX.X, op=ALU.add), reads=[x], writes=[st])
    S.op('dve', lambda e: e.tensor_scalar(out=st[:, 0:1], in0=st[:, 0:1], scalar1=-1.0 / D, scalar2=None, op0=ALU.mult), reads=[st], writes=[st])
    S.op('dve', lambda e: e.tensor_scalar(out=scr[:], in0=xs, scalar1=st[:, 0:1], scalar2=None, op0=ALU.add), reads=[x, st], writes=[scr])
    S.op('dve', lambda e: e.tensor_tensor(out=outs, in0=scr[:], in1=scr[:], op=ALU.mult), reads=[scr], writes=[out])
    S.op('dve', lambda e: e.tensor_reduce(out=st[:, 1:2], in_=outs, axis=AX.X, op=ALU.add), reads=[out], writes=[st])
    S.op('dve', lambda e: e.tensor_scalar(out=st[:, 1:2], in0=st[:, 1:2], scalar1=1.0 / D, scalar2=eps, op0=ALU.mult, op1=ALU.add), reads=[st], writes=[st])
    S.op('act', lambda e: e.activation(out=st[:, 1:2], in_=st[:, 1:2], func=AF.Sqrt), reads=[st], writes=[st])
    S.op('dve', lambda e: e.reciprocal(out=st[:, 1:2], in_=st[:, 1:2]), reads=[st], writes=[st])
    S.op('dve', lambda e: e.scalar_tensor_tensor(out=outs, in0=scr[:], scalar=st[:, 1:2], in1=g_bc[:], op0=ALU.mult, op1=ALU.mult), reads=[scr, st, g_bc], writes=[out])
    S.op('dve', lambda e: e.tensor_tensor(out=outs, in0=outs, in1=b_bc[:], op=ALU.add), reads=[out, b_bc], writes=[out])


def phase_merge(S, ya_d, yb_d, yc_d, proj_d, x_d, wb_ap, wo_ap, g1_ap, b1_ap, wr_ap, rb_ap,
                x1_d, x1T_d, G_d, ident, PS, SEQ, NGRP):
    TG = 256
    NSUB = 2
    NE = NGRP * 8
    NR = NGRP + NE
    with S.scope():
        g_bc = bcast_row(S, "m_g", g1_ap, D)
        b_bc = bcast_row(S, "m_b", b1_ap, D)
        rb_bc = bcast_row(S, "m_rb", rb_ap, NR)
        wr = S.sb("m_wr", [128, 16, NR])
        S.dma('sp', wr[:], wr_ap.rearrange("(p k) c -> p k c", k=16), reads=[], writes=[wr])
        yT = S.sb("m_yT", [128, 20, TG])
        yin = [S.sb(f"m_yin{i}", [128, 2560]) for i in range(2)]
        merged = S.sb("m_merged", [128, NSUB, D])
        mT = S.sb("m_mT", [128, 16, TG])
        wblk = [S.sb(f"m_w{i}", [128, 16, 512]) for i in range(2)]
        gt = [S.sb(f"m_gt{i}", [128, 512]) for i in range(2)]
        tmp = S.sb("m_tmp", [128, 512])
        scr = S.sb("m_scr", [128, D])
        x1 = S.sb("m_x1", [128, D])
        x1T = S.sb("m_x1T", [128, 16, 128])
        st = S.sb("m_st", [128, 4])
        lg = S.sb("m_lg", [128, NR])
        r8 = S.sb("m_r8", [128, 8 * 8])
        sm = S.sb("m_sm", [128, 16])
        oh = S.sb("m_oh", [128, 3, 8])
        mx8 = S.sb("m_mx8", [128, 8])
        sel = S.sb("m_sel", [128, 8])
        gin = S.sb("m_gin", [128, 8])
        G = S.sb("m_G", [128, NE])
        wcnt = 0
        gcnt = 0
        pcnt = 0
        for tg in range(SEQ // TG):
            t0 = tg * TG
            for sub in range(NSUB):
                yi = yin[sub % 2]
                r0 = t0 + sub * 128
                S.dma('sp', yi[:, 0:1024], ya_d.ap()[r0:r0 + 128, :], reads=[ya_d], writes=[yi])
                S.dma('act', yi[:, 1024:2048], yb_d.ap()[r0:r0 + 128, :], reads=[yb_d], writes=[yi])
                S.dma('sp', yi[:, 2048:2560], yc_d.ap()[r0:r0 + 128, :], reads=[yc_d], writes=[yi])
                for kg in range(5):
                    p = PS[6 + kg % 2]
                    for j in range(4):
                        k = kg * 4 + j
                        S.op('pe', lambda e: e.transpose(p[:, j * 128:(j + 1) * 128], yi[:, k * 128:(k + 1) * 128], ident[:]), reads=[yi, ident], writes=[p])
                    S.op('dve', lambda e: e.tensor_copy(yT[:, kg * 4:kg * 4 + 4, sub * 128:(sub + 1) * 128], p[:].rearrange("p (j t) -> p j t", j=4)), reads=[p], writes=[yT])
            for n in range(4):
                for br, (koff, kc) in enumerate(((0, 8), (8, 8), (16, 4))):
                    wb = wblk[wcnt % 2]
                    wcnt += 1
                    S.dma('sp', wb[:, 0:kc, :], wb_ap[koff * 128:(koff + kc) * 128, n * 512:(n + 1) * 512].rearrange("(k p) c -> p k c", p=128), reads=[], writes=[wb])
                    for sub in range(NSUB):
                        a = PS[pcnt % 4]
                        pcnt += 1
                        g = gt[gcnt % 2]
                        gcnt += 1
                        r0 = t0 + sub * 128
                        c0 = OFF_G + br * D + n * 512
                        S.dma('act', g[:], proj_d.ap()[r0:r0 + 128, c0:c0 + 512], reads=[proj_d], writes=[g])
                        S.op('act', lambda e: e.activation(out=g[:], in_=g[:], func=AF.Sigmoid), reads=[g], writes=[g])
                        for k in range(kc):
                            S.op('pe', lambda e: e.matmul(a[:], yT[:, koff + k, sub * 128:(sub + 1) * 128], wb[:, k, :], start=(k == 0), stop=(k == kc - 1)), reads=[yT, wb], writes=[a])
                        if br == 0:
                            S.op('dve', lambda e: e.tensor_tensor(out=merged[:, sub, n * 512:(n + 1) * 512], in0=a[:], in1=g[:], op=ALU.mult), reads=[a, g], writes=[merged])
                        else:
                            S.op('dve', lambda e: e.tensor_tensor(out=tmp[:], in0=a[:], in1=g[:], op=ALU.mult), reads=[a, g], writes=[tmp])
                            S.op('dve', lambda e: e.tensor_tensor(out=merged[:, sub, n * 512:(n + 1) * 512], in0=merged[:, sub, n * 512:(n + 1) * 512], in1=tmp[:], op=ALU.add), reads=[merged, tmp], writes=[merged])
            for sub in range(NSUB):
                for kg in range(4):
                    p = PS[6 + kg % 2]
                    for j in range(4):
                        k = kg * 4 + j
                        S.op('pe', lambda e: e.transpose(p[:, j * 128:(j + 1) * 128], merged[:, sub, k * 128:(k + 1) * 128], ident[:]), reads=[merged, ident], writes=[p])
                    S.op('dve', lambda e: e.tensor_copy(mT[:, kg * 4:kg * 4 + 4, sub * 128:(sub + 1) * 128], p[:].rearrange("p (j t) -> p j t", j=4)), reads=[p], writes=[mT])
            for n in range(4):
                wb = wblk[wcnt % 2]
                wcnt += 1
                S.dma('sp', wb[:], wo_ap[:, n * 512:(n + 1) * 512].rearrange("(k p) c -> p k c", p=128), reads=[], writes=[wb])
                for sub in range(NSUB):
                    a = PS[pcnt % 4]
                    pcnt += 1
                    g = gt[gcnt % 2]
                    gcnt += 1
                    r0 = t0 + sub * 128
                    S.dma('act', g[:], x_d.ap()[r0:r0 + 128, n * 512:(n + 1) * 512], reads=[x_d], writes=[g])
                    for k in range(16):
                        S.op('pe', lambda e: e.matmul(a[:], mT[:, k, sub * 128:(sub + 1) * 128], wb[:, k, :], start=(k == 0), stop=(k == 15)), reads=[mT, wb], writes=[a])
                    S.op('dve', lambda e: e.scalar_tensor_tensor(out=merged[:, sub, n * 512:(n + 1) * 512], in0=g[:], scalar=ALPHA, in1=a[:], op0=ALU.mult, op1=ALU.add), reads=[g, a], writes=[merged])
            for sub in range(NSUB):
                r0 = t0 + sub * 128
                layer_norm(S, merged, merged[:, sub, :], g_bc, b_bc, x1, x1[:], scr, st)
                S.dma('sp', x1_d.ap()[r0:r0 + 128, :], x1[:], reads=[x1], writes=[x1_d])
                for kg in range(4):
                    p = PS[6 + kg % 2]
                    for j in range(4):
                        k = kg * 4 + j
                        S.op('pe', lambda e: e.transpose(p[:, j * 128:(j + 1) * 128], x1[:].rearrange("t (p k) -> t k p", k=16)[:, k, :], ident[:]), reads=[x1, ident], writes=[p])
                    S.op('dve', lambda e: e.tensor_copy(x1T[:, kg * 4:kg * 4 + 4, :], p[:].rearrange("p (j t) -> p j t", j=4)), reads=[p], writes=[x1T])
                S.dma('sp', x1T_d.ap()[:, :, r0:r0 + 128].rearrange("k p t -> p k t"), x1T[:], reads=[x1T], writes=[x1T_d])
                a = PS[pcnt % 4]
                pcnt += 1
                for k in range(16):
                    S.op('pe', lambda e: e.matmul(a[:, 0:NR], x1T[:, k, :], wr[:, k, :], start=(k == 0), stop=(k == 15)), reads=[x1T, wr], writes=[a])
                S.op('dve', lambda e: e.tensor_tensor(out=lg[:], in0=a[:, 0:NR], in1=rb_bc[:], op=ALU.add), reads=[a, rb_bc], writes=[lg])
                S.op('dve', lambda e: e.tensor_reduce(out=sm[:, 0:1], in_=lg[:, 0:NGRP], axis=AX.X, op=ALU.max), reads=[lg], writes=[sm])
                S.op('dve', lambda e: e.tensor_scalar(out=sm[:, 1:2], in0=sm[:, 0:1], scalar1=-1.0, scalar2=None, op0=ALU.mult), reads=[sm], writes=[sm])
                S.op('act', lambda e: e.activation(out=oh[:, 2, 0:NGRP], in_=lg[:, 0:NGRP], func=AF.Exp, bias=sm[:, 1:2], scale=1.0), reads=[lg, sm], writes=[oh])
                S.op('dve', lambda e: e.tensor_reduce(out=sm[:, 2:3], in_=oh[:, 2, 0:NGRP], axis=AX.X, op=ALU.add), reads=[oh], writes=[sm])
                S.op('dve', lambda e: e.reciprocal(out=sm[:, 2:3], in_=sm[:, 2:3]), reads=[sm], writes=[sm])
                S.op('dve', lambda e: e.tensor_scalar(out=oh[:, 2, 0:NGRP], in0=lg[:, 0:NGRP], scalar1=sm[:, 0:1], scalar2=None, op0=ALU.is_equal), reads=[lg, sm], writes=[oh])
                lev = lg[:, NGRP:NR].rearrange("p (g e) -> p g e", e=8)
                S.op('dve', lambda e: e.tensor_tensor(out=r8[:, 0:NE].rearrange("p (g e) -> p g e", e=8), in0=lev, in1=oh[:, 2, 0:NGRP].unsqueeze(2).broadcast_to([128, NGRP, 8]), op=ALU.mult), reads=[lg, oh], writes=[r8])
                S.op('dve', lambda e: e.tensor_reduce(out=sel[:], in_=r8[:, 0:NE].rearrange("p (g e) -> p e g", e=8), axis=AX.X, op=ALU.add), reads=[r8], writes=[sel])
                S.op('dve', lambda e: e.max(out=mx8[:], in_=sel[:]), reads=[sel], writes=[mx8])
                S.op('dve', lambda e: e.tensor_scalar(out=oh[:, 0, :], in0=sel[:], scalar1=mx8[:, 0:1], scalar2=None, op0=ALU.is_equal), reads=[sel, mx8], writes=[oh])
                S.op('dve', lambda e: e.tensor_scalar(out=oh[:, 1, :], in0=sel[:], scalar1=mx8[:, 1:2], scalar2=None, op0=ALU.is_equal), reads=[sel, mx8], writes=[oh])
                S.op('dve', lambda e: e.tensor_tensor(out=sm[:, 3:4], in0=mx8[:, 1:2], in1=mx8[:, 0:1], op=ALU.subtract), reads=[mx8], writes=[sm])
                S.op('act', lambda e: e.activation(out=sm[:, 4:5], in_=sm[:, 3:4], func=AF.Exp), reads=[sm], writes=[sm])
                S.op('dve', lambda e: e.tensor_scalar(out=sm[:, 5:6], in0=sm[:, 4:5], scalar1=1.0, scalar2=None, op0=ALU.add), reads=[sm], writes=[sm])
                S.op('dve', lambda e: e.reciprocal(out=sm[:, 5:6], in_=sm[:, 5:6]), reads=[sm], writes=[sm])
                S.op('dve', lambda e: e.tensor_tensor(out=sm[:, 6:7], in0=sm[:, 5:6], in1=sm[:, 2:3], op=ALU.mult), reads=[sm], writes=[sm])
                S.op('dve', lambda e: e.tensor_tensor(out=sm[:, 7:8], in0=sm[:, 6:7], in1=sm[:, 4:5], op=ALU.mult), reads=[sm], writes=[sm])
                S.op('dve', lambda e: e.tensor_scalar(out=gin[:], in0=oh[:, 0, :], scalar1=sm[:, 6:7], scalar2=None, op0=ALU.mult), reads=[oh, sm], writes=[gin])
                S.op('dve', lambda e: e.scalar_tensor_tensor(out=gin[:], in0=oh[:, 1, :], scalar=sm[:, 7:8], in1=gin[:], op0=ALU.mult, op1=ALU.add), reads=[oh, sm, gin], writes=[gin])
                S.op('dve', lambda e: e.tensor_tensor(out=G[:].rearrange("p (g e) -> p g e", e=8), in0=oh[:, 2, 0:NGRP].unsqueeze(2).broadcast_to([128, NGRP, 8]), in1=gin[:].unsqueeze(1).broadcast_to([128, NGRP, 8]), op=ALU.mult), reads=[oh, gin], writes=[G])
                S.dma('sp', G_d.ap()[r0:r0 + 128, :], G[:], reads=[G], writes=[G_d])


def phase_moe(S, x1_d, x1T_d, G_d, wg_ap, wu_ap, wd_ap, g2_ap, b2_ap, xo_d, PS, SEQ, NGRP):
    TG = 512
    NE = NGRP * 8
    with S.scope():
        g_bc = bcast_row(S, "e_g", g2_ap, D)
        b_bc = bcast_row(S, "e_b", b2_ap, D)
        xT = S.sb("e_xT", [128, 16, TG])
        yacc = S.sb("e_yacc", [128, 4, D])
        G = S.sb("e_G", [128, 4, NE])
        Wg = S.sb("e_Wg", [128, 16, FF])
        Wu = S.sb("e_Wu", [128, 16, FF])
        Wd = S.sb("e_Wd", [128, 3, D])
        hT = S.sb("e_hT", [128, 3, TG])
        gs = [S.sb(f"e_gs{i}", [128, TG]) for i in range(2)]
        x1t = [S.sb(f"e_x1{i}", [128, D]) for i in range(2)]
        scr = S.sb("e_scr", [128, D])
        st = S.sb("e_st", [128, 4])
        pc = 0
        for tg in range(SEQ // TG):
            t0 = tg * TG
            S.dma('sp', xT[:], x1T_d.ap()[:, :, t0:t0 + TG].rearrange("k p t -> p k t"), reads=[x1T_d], writes=[xT])
            S.dma('sp', G[:], G_d.ap()[t0:t0 + TG, :].rearrange("(s p) e -> p s e", p=128), reads=[G_d], writes=[G])
            S.op('dve', lambda e: e.memset(yacc[:], 0.0), writes=[yacc])
            for ex in range(NE):
                S.dma('sp', Wg[:], wg_ap[ex].rearrange("(p k) f -> p k f", k=16), reads=[], writes=[Wg])
                S.dma('act', Wu[:], wu_ap[ex].rearrange("(p k) f -> p k f", k=16), reads=[], writes=[Wu])
                S.dma('sp', Wd[:], wd_ap[ex].rearrange("(k p) c -> p k c", p=128), reads=[], writes=[Wd])
                for f in range(3):
                    a = PS[pc % 8]
                    pc += 1
                    g = gs[f % 2]
                    for k in range(16):
                        S.op('pe', lambda e: e.matmul(a[:], Wg[:, k, f * 128:(f + 1) * 128], xT[:, k, :], start=(k == 0), stop=(k == 15)), reads=[Wg, xT], writes=[a])
                    S.op('act', lambda e: e.activation(out=g[:], in_=a[:], func=AF.Silu), reads=[a], writes=[g])
                    a2 = PS[pc % 8]
                    pc += 1
                    for k in range(16):
                        S.op('pe', lambda e: e.matmul(a2[:], Wu[:, k, f * 128:(f + 1) * 128], xT[:, k, :], start=(k == 0), stop=(k == 15)), reads=[Wu, xT], writes=[a2])
                    S.op('dve', lambda e: e.tensor_tensor(out=hT[:, f, :], in0=a2[:], in1=g[:], op=ALU.mult), reads=[a2, g], writes=[hT])
                for sub in range(4):
                    for n in range(4):
                        a = PS[pc % 8]
                        pc += 1
                        for f in range(3):
                            S.op('pe', lambda e: e.matmul(a[:], hT[:, f, sub * 128:(sub + 1) * 128], Wd[:, f, n * 512:(n + 1) * 512], start=(f == 0), stop=(f == 2)), reads=[hT, Wd], writes=[a])
                        S.op('dve', lambda e: e.scalar_tensor_tensor(out=yacc[:, sub, n * 512:(n + 1) * 512], in0=a[:], scalar=G[:, sub, ex:ex + 1], in1=yacc[:, sub, n * 512:(n + 1) * 512], op0=ALU.mult, op1=ALU.add), reads=[a, G, yacc], writes=[yacc])
            for sub in range(4):
                r0 = t0 + sub * 128
                xi = x1t[sub % 2]
                S.dma('sp', xi[:], x1_d.ap()[r0:r0 + 128, :], reads=[x1_d], writes=[xi])
                S.op('dve', lambda e: e.scalar_tensor_tensor(out=yacc[:, sub, :], in0=xi[:], scalar=ALPHA, in1=yacc[:, sub, :], op0=ALU.mult, op1=ALU.add), reads=[xi, yacc], writes=[yacc])
                layer_norm(S, yacc, yacc[:, sub, :], g_bc, b_bc, xi, xi[:], scr, st)
                S.dma('sp', xo_d.ap()[r0:r0 + 128, :], xi[:], reads=[xi], writes=[xo_d])


def phase_dilated(S, proj_d, nz_d, yc_d, bt_d, ident, PS, SEQ):
    with S.scope():
        qT = S.sb("c_qT", [128, SEQ])
        kT = S.sb("c_kT", [128, SEQ])
        tl = [S.sb(f"c_tl{i}", [128, 128]) for i in range(4)]
        BT = S.sb("c_BT", [128, 2, 256])
        sb_s = [S.sb(f"c_s{i}", [128, 256]) for i in range(2)]
        pT = [S.sb(f"c_pT{i}", [128, 256]) for i in range(2)]
        o_sb = [S.sb(f"c_o{i}", [128, 129]) for i in range(2)]
        tcnt = 0
        scnt = 0
        ocnt = 0
        for g in range(3):
            d = C_DIL[g]
            n = SEQ // d
            nt = n // 128
            V = S.sb(f"c_V{g}", [128, d * (nt + 1), 129])
            S.op('dve', lambda e: e.memset(V[:], 1.0), writes=[V])
            for h in range(4):
                hh = g * 4 + h
                S.dma('sp', BT[:], bt_d.ap()[hh].rearrange("a p f -> p a f"), reads=[bt_d], writes=[BT])
                pv = proj_d.ap().rearrange("(m r) c -> r m c", r=d)
                for r in range(d):
                    for i in range(nt):
                        for (dst, c0) in ((qT, OFF_C + hh * 128), (kT, OFF_C + C_QKV + hh * 128)):
                            tt = tl[tcnt % 4]
                            p = PS[6 + tcnt % 2]
                            tcnt += 1
                            S.dma('sp' if tcnt % 2 else 'act', tt[:], pv[r, i * 128:(i + 1) * 128, c0:c0 + 128], reads=[proj_d], writes=[tt])
                            S.op('pe', lambda e: e.transpose(p[:, 0:128], tt[:], ident[:]), reads=[tt, ident], writes=[p])
                            S.op('dve', lambda e: e.tensor_copy(dst[:, r * n + i * 128:r * n + (i + 1) * 128], p[:, 0:128]), reads=[p], writes=[dst])
                    cv = OFF_C + 2 * C_QKV + hh * 128
                    S.dma('sp', V[0:64, r * (nt + 1), 0:128], pv[r, 0:64, cv:cv + 128], reads=[proj_d], writes=[V])
                    if nt > 1:
                        S.dma('sp', V[:, r * (nt + 1) + 1:r * (nt + 1) + nt, 0:128], pv[r, 64:64 + (nt - 1) * 128, cv:cv + 128].rearrange("(t p) c -> p t c", p=128), reads=[proj_d], writes=[V])
                    S.dma('sp', V[0:64, r * (nt + 1) + nt, 0:128], pv[r, n - 64:n, cv:cv + 128], reads=[proj_d], writes=[V])
                for r in range(d):
                    for mt in range(-1, nt):
                        if mt == -1:
                            k0, npk, bsel, f0, nq, q0 = 0, 64, 1, 128, 128, 0
                        elif mt == nt - 1:
                            k0, npk, bsel, f0, nq, q0 = n - 64, 64, 0, 0, 128, mt * 128
                        else:
                            k0, npk, bsel, f0, nq, q0 = mt * 128 + 64, 128, 0, 0, 256, mt * 128
                        sp_ = PS[4 + scnt % 2]
                        ss_ = sb_s[scnt % 2]
                        pt = pT[scnt % 2]
                        scnt += 1
                        S.op('pe', lambda e: e.matmul(sp_[0:npk, 0:nq], kT[:, r * n + k0:r * n + k0 + npk], qT[:, r * n + q0:r * n + q0 + nq], start=True, stop=True), reads=[kT, qT], writes=[sp_])
                        S.op('dve', lambda e: e.scalar_tensor_tensor(out=ss_[0:npk, 0:nq], in0=sp_[0:npk, 0:nq], scalar=128.0 ** -0.5, in1=BT[0:npk, bsel, f0:f0 + nq], op0=ALU.mult, op1=ALU.add), reads=[sp_, BT], writes=[ss_])
                        S.op('act', lambda e: e.activation(out=pt[0:npk, 0:nq], in_=ss_[0:npk, 0:nq], func=AF.Exp), reads=[ss_], writes=[pt])
                        vt = V[0:npk, r * (nt + 1) + mt + 1, :]
                        for fb in range(nq // 128):
                            qb = q0 // 128 + fb
                            first = (mt == qb - 1)
                            acc = PS[qb % 2]
                            S.op('pe', lambda e: e.matmul(acc[:, 0:129], pt[0:npk, fb * 128:(fb + 1) * 128], vt, start=first, stop=(not first)), reads=[pt, V], writes=[acc])
                            if not first:
                                o = o_sb[ocnt % 2]
                                ocnt += 1
                                S.op('dve', lambda e: e.tensor_copy(o[:], acc[:, 0:129]), reads=[acc], writes=[o])
                                dst = nz_d.ap()[g].rearrange("(m r) h c -> r m h c", r=d)[r, qb * 128:(qb + 1) * 128, h, :]
                                S.dma('sp', dst, o[:], reads=[o], writes=[nz_d])
        nzt = [S.sb(f"c_nz{i}", [128, 3, 4, 129]) for i in range(2)]
        yo = [S.sb(f"c_yo{i}", [128, 4, 128]) for i in range(2)]
        acc4 = S.sb("c_acc4", [128, 4, 129])
        for i in range(SEQ // 128):
            t = nzt[i % 2]
            y = yo[i % 2]
            S.dma('sp', t[:], nz_d.ap()[:, i * 128:(i + 1) * 128, :, :].rearrange("g p h c -> p g h c"), reads=[nz_d], writes=[t])
            S.op('dve', lambda e: e.tensor_tensor(out=acc4[:], in0=t[:, 0], in1=t[:, 1], op=ALU.add), reads=[t], writes=[acc4])
            S.op('dve', lambda e: e.tensor_tensor(out=acc4[:], in0=acc4[:], in1=t[:, 2], op=ALU.add), reads=[t, acc4], writes=[acc4])
            S.op('dve', lambda e: e.reciprocal(out=acc4[:, :, 128:129], in_=acc4[:, :, 128:129]), reads=[acc4], writes=[acc4])
            S.op('dve', lambda e: e.tensor_tensor(out=y[:], in0=acc4[:, :, 0:128], in1=acc4[:, :, 128:129].broadcast_to([128, 4, 128]), op=ALU.mult), reads=[acc4], writes=[y])
            S.dma('sp', yc_d.ap()[i * 128:(i + 1) * 128, :], y[:].rearrange("p h c -> p (h c)"), reads=[y], writes=[yc_d])


def phase_rwkv(S, proj_d, yb_d, Xd, VTd, Od, bon_d, gg_d, prm, sel_d, ident, J, PS, SEQ, parts=(1, 1, 1)):
    NT = SEQ // 128
    NB = 3456
    if parts[0]:
      with S.scope():
        y = S.sb("b_y", [128, NB])
        pv = S.sb("b_pv", [128, NB])
        nx = S.sb("b_nx", [128, NB])
        mup = bcast_row(S, "b_mup", prm['mu_prev'], NB)
        mun = bcast_row(S, "b_mun", prm['mu_next'], NB, q='act')
        w0 = [bcast_row(S, f"b_w0{d}", prm['w0'][d], 1024) for d in range(2)]
        a0 = [bcast_row(S, f"b_a0{d}", prm['a0'][d], 1024, q='act') for d in range(2)]
        kkb = bcast_row(S, "b_kkb", prm['k_k'], 1024)
        kab = bcast_row(S, "b_kab", prm['k_a'], 1024)
        rkb = bcast_row(S, "b_rkb", prm['r_k'], 1024)
        omka = S.sb("b_omka", [128, 1024])
        S.op('dve', lambda e: e.tensor_scalar(out=omka[:], in0=kab[:], scalar1=-1.0, scalar2=1.0, op0=ALU.mult, op1=ALU.add), reads=[kab], writes=[omka])
        w2t = S.sb("b_w2t", [128, 1024])
        a2t = S.sb("b_a2t", [128, 1024])
        g2t = S.sb("b_g2t", [128, 1024])
        S.dma('sp', w2t[:], prm['w2'].rearrange("d r c -> (d r) c"), reads=[], writes=[w2t])
        S.dma('sp', a2t[:], prm['a2'].rearrange("d r c -> (d r) c"), reads=[], writes=[a2t])
        S.dma('sp', g2t[:], prm['g2'], reads=[], writes=[g2t])
        L = S.sb("b_L", [128, 384])
        LT = S.sb("b_LT", [128, 3, 128])
        wd = [S.sb(f"b_wd{d}", [128, 1024]) for d in range(2)]
        ad = [S.sb(f"b_ad{d}", [128, 1024]) for d in range(2)]
        kd = [S.sb(f"b_kd{d}", [128, 1024]) for d in range(2)]
        bd = [S.sb(f"b_bd{d}", [128, 1024]) for d in range(2)]
        kk = S.sb("b_kk", [128, 1024])
        gg = S.sb("b_gg", [128, 1024])
        tmp = S.sb("b_tmp", [128, 1024])
        ft = [S.sb(f"b_ft{i}", [128, 1024]) for i in range(2)]
        vt = [S.sb(f"b_vt{i}", [128, 2, 8, 64]) for i in range(2)]
        ss = S.sb("b_ss", [128, 16])
        pc = 0
        fc = 0
        pbv = proj_d.ap()[:, OFF_B:OFF_B + NB]
        for i in range(NT):
            r0 = i * 128
            S.dma('sp', y[:], pbv[r0:r0 + 128, :], reads=[proj_d], writes=[y])
            if i == 0:
                S.op('dve', lambda e: e.memset(pv[:], 0.0), writes=[pv])
                S.dma('act', pv[1:128, :], pbv[0:127, :], reads=[proj_d], writes=[pv])
            else:
                S.dma('act', pv[:], pbv[r0 - 1:r0 + 127, :], reads=[proj_d], writes=[pv])
            if i == NT - 1:
                S.op('dve', lambda e: e.memset(nx[:], 0.0), writes=[nx])
                S.dma('sp', nx[0:127, :], pbv[r0 + 1:r0 + 128, :], reads=[proj_d], writes=[nx])
            else:
                S.dma('sp', nx[:], pbv[r0 + 1:r0 + 129, :], reads=[proj_d], writes=[nx])
            tt = lambda o, a, b, op: S.op('dve', lambda e: e.tensor_tensor(out=o[:], in0=a[:], in1=b[:], op=op), reads=[a, b], writes=[o])
            tt(pv, pv, y, ALU.subtract)
            tt(pv, pv, mup, ALU.mult)
            tt(nx, nx, y, ALU.subtract)
            tt(nx, nx, mun, ALU.mult)
            tt(y, y, pv, ALU.add)
            tt(y, y, nx, ALU.add)
            r_ = y[:, 0:1024]
            k_ = y[:, 1024:2048]
            v_ = y[:, 2048:3072]
            S.op('act', lambda e: e.activation(out=L[:, 0:128], in_=y[:, 3072:3200], func=AF.Tanh), reads=[y], writes=[L])
            S.op('act', lambda e: e.copy(L[:, 128:256], y[:, 3200:3328]), reads=[y], writes=[L])
            S.op('act', lambda e: e.activation(out=L[:, 256:384], in_=y[:, 3328:3456], func=AF.Sigmoid), reads=[y], writes=[L])
            p = PS[6]
            for j in range(3):
                S.op('pe', lambda e: e.transpose(p[:, j * 128:(j + 1) * 128], L[:, j * 128:(j + 1) * 128], ident[:]), reads=[L, ident], writes=[p])
            S.op('dve', lambda e: e.tensor_copy(LT[:].rearrange("p j t -> p (j t)"), p[:, 0:384]), reads=[p], writes=[LT])
            for d in range(2):
                for half in range(2):
                    cs = slice(half * 512, (half + 1) * 512)
                    a = PS[pc % 4]
                    pc += 1
                    S.op('pe', lambda e: e.matmul(a[:], LT[d * 64:(d + 1) * 64, 0, :], w2t[d * 64:(d + 1) * 64, cs], start=True, stop=True), reads=[LT, w2t], writes=[a])
                    S.op('dve', lambda e: e.tensor_tensor(out=wd[d][:, cs], in0=a[:], in1=w0[d][:, cs], op=ALU.add), reads=[a, w0[d]], writes=[wd[d]])
                    a = PS[pc % 4]
                    pc += 1
                    S.op('pe', lambda e: e.matmul(a[:], LT[d * 64:(d + 1) * 64, 1, :], a2t[d * 64:(d + 1) * 64, cs], start=True, stop=True), reads=[LT, a2t], writes=[a])
                    S.op('dve', lambda e: e.tensor_tensor(out=ad[d][:, cs], in0=a[:], in1=a0[d][:, cs], op=ALU.add), reads=[a, a0[d]], writes=[ad[d]])
                S.op('act', lambda e: e.activation(out=wd[d][:], in_=wd[d][:], func=AF.Sigmoid), reads=[wd[d]], writes=[wd[d]])
                S.op('act', lambda e: e.activation(out=wd[d][:], in_=wd[d][:], func=AF.Exp, scale=-float(np.exp(-0.5))), reads=[wd[d]], writes=[wd[d]])
                S.op('act', lambda e: e.activation(out=ad[d][:], in_=ad[d][:], func=AF.Sigmoid), reads=[ad[d]], writes=[ad[d]])
            for half in range(2):
                cs = slice(half * 512, (half + 1) * 512)
                a = PS[pc % 4]
                pc += 1
                S.op('pe', lambda e: e.matmul(a[:], LT[:, 2, :], g2t[:, cs], start=True, stop=True), reads=[LT, g2t], writes=[a])
                S.op('act', lambda e: e.copy(gg[:, cs], a[:]), reads=[a], writes=[gg])
            S.dma('act', gg_d.ap()[r0:r0 + 128, :], gg[:], reads=[gg], writes=[gg_d])
            S.op('dve', lambda e: e.tensor_tensor(out=kk[:], in0=k_, in1=kkb[:], op=ALU.mult), reads=[y, kkb], writes=[kk])
            S.op('dve', lambda e: e.tensor_tensor(out=tmp[:], in0=kk[:], in1=kk[:], op=ALU.mult), reads=[kk], writes=[tmp])
            S.op('dve', lambda e: e.tensor_reduce(out=ss[:], in_=tmp[:].rearrange("p (h k) -> p h k", k=64), axis=AX.X, op=ALU.add), reads=[tmp], writes=[ss])
            S.op('dve', lambda e: e.tensor_scalar(out=ss[:], in0=ss[:], scalar1=1e-12, scalar2=None, op0=ALU.add), reads=[ss], writes=[ss])
            S.op('act', lambda e: e.activation(out=ss[:], in_=ss[:], func=AF.Sqrt), reads=[ss], writes=[ss])
            S.op('dve', lambda e: e.reciprocal(out=ss[:], in_=ss[:]), reads=[ss], writes=[ss])
            S.op('dve', lambda e: e.tensor_tensor(out=kk[:].rearrange("p (h k) -> p h k", k=64), in0=kk[:].rearrange("p (h k) -> p h k", k=64), in1=ss[:].unsqueeze(2).broadcast_to([128, 16, 64]), op=ALU.mult), reads=[kk, ss], writes=[kk])
            for d in range(2):
                tt(kd[d], ad[d], kab, ALU.mult)
                tt(kd[d], kd[d], omka, ALU.add)
                S.op('dve', lambda e: e.tensor_tensor(out=kd[d][:], in0=kd[d][:], in1=k_, op=ALU.mult), reads=[kd[d], y], writes=[kd[d]])
                tt(bd[d], kk, ad[d], ALU.mult)
                S.op('dve', lambda e: e.tensor_scalar(out=bd[d][:], in0=bd[d][:], scalar1=-1.0, scalar2=None, op0=ALU.mult), reads=[bd[d]], writes=[bd[d]])
            tt(tmp, kd[0], kd[1], ALU.add)
            S.op('dve', lambda e: e.tensor_tensor(out=tmp[:], in0=tmp[:], in1=r_, op=ALU.mult), reads=[tmp, y], writes=[tmp])
            tt(tmp, tmp, rkb, ALU.mult)
            S.op('dve', lambda e: e.tensor_reduce(out=ss[:], in_=tmp[:].rearrange("p (h k) -> p h k", k=64), axis=AX.X, op=ALU.add), reads=[tmp], writes=[ss])
            S.op('dve', lambda e: e.tensor_tensor(out=tmp[:].rearrange("p (h k) -> p h k", k=64), in0=y[:, 2048:3072].rearrange("p (h k) -> p h k", k=64), in1=ss[:].unsqueeze(2).broadcast_to([128, 16, 64]), op=ALU.mult), reads=[y, ss], writes=[tmp])
            S.dma('act', bon_d.ap()[r0:r0 + 128, :], tmp[:], reads=[tmp], writes=[bon_d])
            for d in range(2):
                srcs = [(kk, kk[:]), (wd[d], wd[d][:]), (bd[d], bd[d][:]), (kd[d], kd[d][:]), (y, r_)]
                for X, (sb_, sap) in enumerate(srcs):
                    f_ = ft[fc % 2]
                    fc += 1
                    fv = f_[:].rearrange("p (h q k) -> p h q k", h=2, q=8)
                    if d == 0:
                        rr = r0
                        S.op('act', lambda e: e.copy(fv.rearrange("p h q k -> p q h k"), sap.rearrange("p (q h k) -> p q h k", q=8, h=2)), reads=[sb_], writes=[f_])
                    else:
                        rr = (NT - 1 - i) * 128
                        for half in range(2):
                            cs = slice(half * 512, (half + 1) * 512)
                            a = PS[pc % 4]
                            pc += 1
                            S.op('pe', lambda e: e.matmul(a[:], J[:], sap[:, cs], start=True, stop=True), reads=[J, sb_], writes=[a])
                            S.op('act', lambda e: e.copy(fv[:, :, half * 4:(half + 1) * 4, :].rearrange("p h q k -> p q h k"), a[:].rearrange("p (q h k) -> p q h k", q=4, h=2)), reads=[a], writes=[f_])
                    for h2 in range(2):
                        S.dma('sp', Xd.ap()[d, rr:rr + 128, h2, X, :, :], fv[:, h2, :, :], reads=[f_], writes=[Xd])
            for d in range(2):
                v2 = vt[d]
                for qg in range(2):
                    p = PS[4 + qg]
                    for j in range(4):
                        hq = qg * 4 + j
                        if d == 0:
                            S.op('pe', lambda e: e.transpose(p[:, j * 128:(j + 1) * 128], y[:, 2048 + hq * 128:2048 + (hq + 1) * 128], ident[:]), reads=[y, ident], writes=[p])
                        else:
                            S.op('pe', lambda e: e.matmul(p[:, j * 128:(j + 1) * 128], y[:, 2048 + hq * 128:2048 + (hq + 1) * 128], J[:], start=True, stop=True), reads=[y, J], writes=[p])
                    S.op('dve', lambda e: e.tensor_copy(v2[:, :, qg * 4:qg * 4 + 4, :].rearrange("p c j t -> p j c t"), p[:].rearrange("p (j c t) -> p j c t", j=4, c=2)), reads=[p], writes=[v2])
                ti = i if d == 0 else (NT - 1 - i)
                S.dma('sp', VTd.ap()[d, :, 2 * ti:2 * ti + 2, :, :], v2[:], reads=[v2], writes=[VTd])
    if parts[1]:
      with S.scope():
        Sel = S.sb("s_sel", [128, 64, 128])
        S.dma('sp', Sel[:], sel_d.ap(), reads=[sel_d], writes=[Sel])
        R = [S.sb(f"s_R{i}", [128, 2, 5, 512]) for i in range(2)]
        vtc = [S.sb(f"s_vt{i}", [128, 16, 64]) for i in range(2)]
        Ob = [S.sb(f"s_Ob{i}", [128, 16, 64]) for i in range(2)]
        St = S.sb("s_St", [128, 1024])
        t1 = S.sb("s_t1", [128, 1024])
        t2 = S.sb("s_t2", [128, 1024])
        sa = S.sb("s_sa", [128, 16])
        S.op('dve', lambda e: e.memset(St[:], 0.0), writes=[St])
        P2 = PS.P2
        bkc = 0
        v3 = lambda ap: ap.rearrange("p (q k) -> p q k", k=64)
        for c in range(SEQ // 64):
            Rc = R[c % 2]
            vc_ = vtc[c % 2]
            ob = Ob[c % 2]
            for d in range(2):
                for h2 in range(2):
                    S.dma('sp' if d == 0 else 'act', Rc[h2 * 64:(h2 + 1) * 64, d, :, :], Xd.ap()[d, c * 64:(c + 1) * 64, h2, :, :, :].rearrange("t x q k -> t x (q k)"), reads=[Xd], writes=[Rc])
                S.dma('sp', vc_[:, d * 8:(d + 1) * 8, :], VTd.ap()[d, :, c, :, :], reads=[VTd], writes=[vc_])
            for tl_ in range(64):
                bk = []

                def bcast(X):
                    nonlocal bkc
                    b = P2[bkc % 4]
                    bkc += 1
                    for d in range(2):
                        S.op('pe', lambda e: e.matmul(b[:, d * 512:(d + 1) * 512], Sel[:, tl_, :], Rc[:, d, X, :], start=True, stop=True), reads=[Sel, Rc], writes=[b])
                    bk.append(b)
                for X in range(4):
                    bcast(X)
                S.op('dve', lambda e: e.tensor_tensor(out=t1[:], in0=St[:], in1=bk[0][:], op=ALU.mult), reads=[St, bk[0]], writes=[t1])
                bcast(4)
                S.op('dve', lambda e: e.tensor_reduce(out=sa[:], in_=v3(t1[:]), axis=AX.X, op=ALU.add), reads=[t1], writes=[sa])
                S.op('dve', lambda e: e.tensor_tensor(out=St[:], in0=St[:], in1=bk[1][:], op=ALU.mult), reads=[St, bk[1]], writes=[St])
                S.op('dve', lambda e: e.tensor_tensor(out=v3(t2[:]), in0=v3(bk[2][:]), in1=sa[:].unsqueeze(2).broadcast_to([128, 16, 64]), op=ALU.mult), reads=[bk[2], sa], writes=[t2])
                S.op('dve', lambda e: e.tensor_tensor(out=St[:], in0=St[:], in1=t2[:], op=ALU.add), reads=[St, t2], writes=[St])
                S.op('dve', lambda e: e.tensor_tensor(out=v3(t2[:]), in0=v3(bk[3][:]), in1=vc_[:, :, tl_:tl_ + 1].broadcast_to([128, 16, 64]), op=ALU.mult), reads=[bk[3], vc_], writes=[t2])
                S.op('dve', lambda e: e.tensor_tensor(out=St[:], in0=St[:], in1=t2[:], op=ALU.add), reads=[St, t2], writes=[St])
                S.op('dve', lambda e: e.tensor_tensor(out=t1[:], in0=St[:], in1=bk[4][:], op=ALU.mult), reads=[St, bk[4]], writes=[t1])
                for d in range(2):
                    col = tl_ if d == 0 else 63 - tl_
                    S.op('dve', lambda e: e.tensor_reduce(out=ob[:, d * 8:(d + 1) * 8, col:col + 1], in_=v3(t1[:, d * 512:(d + 1) * 512]), axis=AX.X, op=ALU.add), reads=[t1], writes=[ob])
            S.dma('sp', Od.ap()[0, :, c, :, :], ob[:, 0:8, :], reads=[ob], writes=[Od])
            S.dma('sp', Od.ap()[1, :, SEQ // 64 - 1 - c, :, :], ob[:, 8:16, :], reads=[ob], writes=[Od])
    if parts[2]:
      with S.scope():
        gng = bcast_row(S, "f_gng", prm['gn_g'], 1024)
        gnb = bcast_row(S, "f_gnb", prm['gn_b'], 1024)
        Ot = [S.sb(f"f_Ot{i}", [128, 2, 2, 8, 64]) for i in range(2)]
        osum = S.sb("f_osum", [128, 8, 128])
        o = S.sb("f_o", [128, 1024])
        xc = S.sb("f_xc", [128, 1024])
        sq = S.sb("f_sq", [128, 1024])
        bt_ = [S.sb(f"f_bon{i}", [128, 1024]) for i in range(2)]
        gt_ = [S.sb(f"f_g{i}", [128, 1024]) for i in range(2)]
        st = S.sb("f_st", [128, 2, 16])
        h3 = lambda ap: ap.rearrange("p (h k) -> p h k", k=64)
        for i in range(NT):
            r0 = i * 128
            ot = Ot[i % 2]
            bo = bt_[i % 2]
            gg = gt_[i % 2]
            for d in range(2):
                S.dma('sp', ot[:, d], Od.ap()[d, :, 2 * i:2 * i + 2, :, :], reads=[Od], writes=[ot])
            S.dma('act', bo[:], bon_d.ap()[r0:r0 + 128, :], reads=[bon_d], writes=[bo])
            S.dma('act', gg[:], gg_d.ap()[r0:r0 + 128, :], reads=[gg_d], writes=[gg])
            for cc in range(2):
                S.op('dve', lambda e: e.tensor_tensor(out=osum[:, :, cc * 64:(cc + 1) * 64], in0=ot[:, 0, cc], in1=ot[:, 1, cc], op=ALU.add), reads=[ot], writes=[osum])
            for qg in range(2):
                p = PS[qg]
                for j in range(4):
                    hq = qg * 4 + j
                    S.op('pe', lambda e: e.transpose(p[:, j * 128:(j + 1) * 128], osum[:, hq, :], ident[:]), reads=[osum, ident], writes=[p])
                S.op('dve', lambda e: e.tensor_copy(o[:, qg * 512:(qg + 1) * 512], p[:]), reads=[p], writes=[o])
            S.op('dve', lambda e: e.tensor_reduce(out=st[:, 0, :], in_=h3(o[:]), axis=AX.X, op=ALU.add), reads=[o], writes=[st])
            S.op('dve', lambda e: e.tensor_scalar(out=st[:, 0, :], in0=st[:, 0, :], scalar1=-1.0 / 64, scalar2=None, op0=ALU.mult), reads=[st], writes=[st])
            S.op('dve', lambda e: e.tensor_tensor(out=h3(xc[:]), in0=h3(o[:]), in1=st[:, 0, :].unsqueeze(2).broadcast_to([128, 16, 64]), op=ALU.add), reads=[o, st], writes=[xc])
            S.op('dve', lambda e: e.tensor_tensor(out=sq[:], in0=xc[:], in1=xc[:], op=ALU.mult), reads=[xc], writes=[sq])
            S.op('dve', lambda e: e.tensor_reduce(out=st[:, 1, :], in_=h3(sq[:]), axis=AX.X, op=ALU.add), reads=[sq], writes=[st])
            S.op('dve', lambda e: e.tensor_scalar(out=st[:, 1, :], in0=st[:, 1, :], scalar1=1.0 / 64, scalar2=64e-5, op0=ALU.mult, op1=ALU.add), reads=[st], writes=[st])
            S.op('act', lambda e: e.activation(out=st[:, 1, :], in_=st[:, 1, :], func=AF.Sqrt), reads=[st], writes=[st])
            S.op('dve', lambda e: e.reciprocal(out=st[:, 1, :], in_=st[:, 1, :]), reads=[st], writes=[st])
            S.op('dve', lambda e: e.tensor_tensor(out=h3(xc[:]), in0=h3(xc[:]), in1=st[:, 1, :].unsqueeze(2).broadcast_to([128, 16, 64]), op=ALU.mult), reads=[xc, st], writes=[xc])
            S.op('dve', lambda e: e.tensor_tensor(out=xc[:], in0=xc[:], in1=gng[:], op=ALU.mult), reads=[xc, gng], writes=[xc])
            S.op('dve', lambda e: e.tensor_tensor(out=xc[:], in0=xc[:], in1=gnb[:], op=ALU.add), reads=[xc, gnb], writes=[xc])
            S.op('dve', lambda e: e.tensor_tensor(out=xc[:], in0=xc[:], in1=bo[:], op=ALU.add), reads=[xc, bo], writes=[xc])
            S.op('dve', lambda e: e.tensor_tensor(out=bo[:], in0=xc[:], in1=gg[:], op=ALU.mult), reads=[xc, gg], writes=[bo])
            S.dma('sp', yb_d.ap()[r0:r0 + 128, :], bo[:], reads=[bo], writes=[yb_d])


VEC_NAMES = ['mu_prev', 'mu_next', 'rwkv_w0', 'rwkv_w2', 'rwkv_a0', 'rwkv_a2', 'rwkv_g2', 'rwkv_k_k', 'rwkv_k_a', 'rwkv_r_k',
             'rwkv_gn_g', 'rwkv_gn_b', 'q_norm', 'k_norm', 'ln1_g', 'ln1_b', 'ln2_g', 'ln2_b']


def build_full(SEQ, L, NGRP, shapes, dbg=False):
    global ALPHA
    ALPHA = (2.0 * L) ** 0.25
    NE = NGRP * 8
    NR = NGRP + NE
    nc = bass.Bass("TRN2", target_bir_lowering=False)
    with ExitStack() as es:
        S = Sched(nc, es)
        I = lambda n, sh: S.dram(n, list(sh), F32, kind="ExternalInput")
        x_in = I("x", [SEQ, D])
        w_in = I("w_in", [L, D, IN_COLS])
        w_branch = I("w_branch", [L, 2560, D])
        w_out = I("w_out", [L, D, D])
        vec = {n: I(n, shapes[n]) for n in VEC_NAMES}
        wr = I("wr", [L, D, NR])
        rb = I("rb", [L, NR])
        wg = I("w_gate", [L, NE, D, FF])
        wu = I("w_up", [L, NE, D, FF])
        wd = I("w_down", [L, NE, FF, D])
        bt = I("bt", [12, 2, 128, 256])
        cos = I("cos", [SEQ, 128])
        sin = I("sin", [SEQ, 128])
        sel = I("sel", [128, 64, 128])
        identd = I("identd", [128, 128])
        Jd = I("Jd", [128, 128])
        out = S.dram("out", [SEQ, D], F32, kind="ExternalOutput")
        DBG = ("proj", "ya", "yb", "yc", "x1") if dbg else ()
        T = lambda n, sh: S.dram(n, list(sh), F32, kind="ExternalOutput" if n in DBG else "Internal")
        proj = T("proj", [SEQ, IN_COLS])
        qkn = T("qkn", [SEQ, 1280])
        ya = T("ya", [SEQ, 1024])
        yb = T("yb", [SEQ, 1024])
        yc = T("yc", [SEQ, 512])
        nz = T("nz", [3, SEQ, 4, 129])
        Xd = T("Xd", [2, SEQ, 2, 5, 8, 64])
        VTd = T("VTd", [2, 128, SEQ // 64, 8, 64])
        Od = T("Od", [2, 128, SEQ // 64, 8, 64])
        bon = T("bon", [SEQ, 1024])
        ggd = T("ggd", [SEQ, 1024])
        x1 = T("x1", [SEQ, D])
        x1T = T("x1T", [16, 128, SEQ])
        Gd = T("Gd", [SEQ, NE])
        xs = [T("xA", [SEQ, D]), T("xB", [SEQ, D])]
        ident = S.sb("ident", [128, 128])
        J = S.sb("J", [128, 128])
        S.dma('sp', ident[:], identd.ap(), reads=[identd], writes=[ident])
        S.dma('sp', J[:], Jd.ap(), reads=[Jd], writes=[J])
        PS = make_psum(S)
        xc = x_in
        for l in range(L):
            xo = out if l == L - 1 else xs[l % 2]
            _w = _APBuf(w_in, l)
            phase_proj(S, xc, _w, proj, ident, PS, SEQ)
            phase_attn_a(S, proj, ya, qkn, cos, sin, vec['q_norm'].ap()[l], vec['k_norm'].ap()[l], ident, PS, SEQ)
            prm = dict(mu_prev=vec['mu_prev'].ap()[l], mu_next=vec['mu_next'].ap()[l], w0=vec['rwkv_w0'].ap()[l], w2=vec['rwkv_w2'].ap()[l],
                       a0=vec['rwkv_a0'].ap()[l], a2=vec['rwkv_a2'].ap()[l], g2=vec['rwkv_g2'].ap()[l], k_k=vec['rwkv_k_k'].ap()[l],
                       k_a=vec['rwkv_k_a'].ap()[l], r_k=vec['rwkv_r_k'].ap()[l], gn_g=vec['rwkv_gn_g'].ap()[l], gn_b=vec['rwkv_gn_b'].ap()[l])
            phase_rwkv(S, proj, yb, Xd, VTd, Od, bon, ggd, prm, sel, ident, J, PS, SEQ)
            phase_dilated(S, proj, nz, yc, bt, ident, PS, SEQ)
            phase_merge(S, ya, yb, yc, proj, xc, w_branch.ap()[l], w_out.ap()[l], vec['ln1_g'].ap()[l], vec['ln1_b'].ap()[l],
                        wr.ap()[l], rb.ap()[l], x1, x1T, Gd, ident, PS, SEQ, NGRP)
            phase_moe(S, x1, x1T, Gd, wg.ap()[l], wu.ap()[l], wd.ap()[l], vec['ln2_g'].ap()[l], vec['ln2_b'].ap()[l], xo, PS, SEQ, NGRP)
            xc = xo
        S.finish([out], 'sp')
        S.barrier()
        print("ninst", S.ninst, flush=True)
    return nc


class _APBuf:
    def __init__(self, buf, l):
        self.buf = buf
        self.l = l
        self.dram = True
        self.w = buf.w
        self.r = buf.r
        self.name = buf.name

    def ap(self):
        return self.buf.ap()[self.l]


def host_inputs(inputs, b, SEQ, L, NGRP):
    f = lambda a: np.ascontiguousarray(a, dtype=np.float32)
    m = {"x": f(inputs["x"][b, :SEQ])}
    for n in ["w_in", "w_branch", "w_out", "w_gate", "w_up", "w_down"] + VEC_NAMES:
        m[n] = f(inputs[n][:L])
    NE = NGRP * 8
    m["w_gate"] = f(inputs["w_gate"][:L, :NE])
    m["w_up"] = f(inputs["w_up"][:L, :NE])
    m["w_down"] = f(inputs["w_down"][:L, :NE])
    m["wr"] = f(np.concatenate([inputs["router_group_w"][:L, :, :NGRP], inputs["router_expert_w"][:L, :, :NE]], axis=2))
    m["rb"] = f(np.concatenate([inputs["router_group_b"][:L, :NGRP], inputs["router_expert_b"][:L, :NE]], axis=1))
    m["bt"] = bias_tables(np.asarray(inputs["rel_bias"], dtype=np.float32))
    COS, SINS = rope_tables(SEQ)
    m["cos"] = COS
    m["sin"] = SINS
    m["sel"] = sel_table()
    m["identd"] = np.eye(128, dtype=np.float32)
    m["Jd"] = anti_ident()
    return m


class _PSList(list):
    pass


def make_psum(S):
    P2 = [S.ps(f"ps2_{j}", [128, 1024]) for j in range(4)]
    PS = _PSList()
    for i in range(8):
        PS.append(Buf(S, P2[i // 2].t[:, (i % 2) * 512:(i % 2 + 1) * 512], f"psv{i}"))
    PS.P2 = P2
    return PS


from concourse.bass_utils import run_bass_kernel_spmd

_SEQ, _L, _NGRP, _NB = 4096, 4, 8, 4


def kernel(**inputs):
    inputs = {k: np.asarray(v) for k, v in inputs.items()}
    shapes = {n: tuple(inputs[n].shape) for n in VEC_NAMES}
    nc = build_full(_SEQ, _L, _NGRP, shapes)
    ims = [host_inputs(inputs, b, _SEQ, _L, _NGRP) for b in range(_NB)]
    res = run_bass_kernel_spmd(nc, ims, core_ids=list(range(_NB)))
    return np.stack([np.asarray(res.results[b]["out"], dtype=np.float32) for b in range(_NB)], axis=0)
```
